# Optimizing a Trainium2 kernel written in Bass

```python
import jax
import jax.numpy as jnp
from jax import lax
import numpy as np

D_MODEL = 2048
BATCH = 4
SEQ = 2048
DEPTH = 1

CTX_LEN = 256
GRID_W = 64
N_MOD = 6
FOUR_GROUPS = 8
FOUR_GROUP_DIM = D_MODEL // 16
FOUR_WIDTH = FOUR_GROUPS * FOUR_GROUP_DIM
RET_HEADS = 8
RET_DK = D_MODEL // 16
RET_DV = D_MODEL // 8
RET_QK_WIDTH = RET_HEADS * RET_DK
RET_V_WIDTH = RET_HEADS * RET_DV
RET_CHUNK = 128
ROPE_BASE = 10000.0
N_GROUPS = 4
EXPERTS_PER_GROUP = 8
N_EXPERTS = N_GROUPS * EXPERTS_PER_GROUP
TOP_K_IN_GROUP = 2
EXPERT_FF = D_MODEL // 4
EPS = 1e-6
Q_OFF = FOUR_WIDTH
K_OFF = Q_OFF + RET_QK_WIDTH
V_OFF = K_OFF + RET_QK_WIDTH
GF_OFF = V_OFF + RET_V_WIDTH
GB_OFF = GF_OFF + RET_V_WIDTH
MF_OFF = GB_OFF + RET_V_WIDTH
MR_OFF = MF_OFF + D_MODEL
IN_WIDTH = MR_OFF + D_MODEL
SPLITS = (Q_OFF, K_OFF, V_OFF, GF_OFF, GB_OFF, MF_OFF, MR_OFF)

kernel_name = 'hybrid_fnet_retnet_hmoe_dit'


def rmsnorm(x, g):
    xf = x.astype(jnp.float32)
    y = xf * lax.rsqrt(jnp.mean(xf * xf, axis=-1, keepdims=True) + EPS)
    return y.astype(x.dtype) * g


def modulate(h, shift, scale):
    return h * (1.0 + scale) + shift


def fourier_mix(u):
    b_, n = u.shape[0], u.shape[1]
    ug = u.reshape(b_, n, FOUR_GROUPS, FOUR_GROUP_DIM).astype(jnp.float32)
    f = jnp.fft.fftn(ug, axes=(1, 3), norm='ortho').real
    return f.reshape(b_, n, FOUR_WIDTH).astype(u.dtype)


def grid_rope_tables(n_tokens):
    rows = n_tokens // GRID_W
    row = jnp.repeat(jnp.arange(rows, dtype=jnp.float32), GRID_W)
    col = jnp.tile(jnp.arange(GRID_W, dtype=jnp.float32), rows)
    n_freq = RET_DK // 4
    inv = ROPE_BASE ** (-jnp.arange(n_freq, dtype=jnp.float32) / n_freq)
    ang = jnp.concatenate([row[:, None] * inv, col[:, None] * inv], axis=-1)
    return jnp.cos(ang), jnp.sin(ang)


def apply_rope(t, cos, sin):
    half = RET_DK // 2
    t1, t2 = t[..., :half], t[..., half:]
    return jnp.concatenate([t1 * cos - t2 * sin, t1 * sin + t2 * cos], axis=-1)


def to_heads(t, d):
    b_, n = t.shape[0], t.shape[1]
    return t.reshape(b_, n, RET_HEADS, d).transpose(0, 2, 1, 3).astype(jnp.float32)


def head_norm(y):
    mu = jnp.mean(y, axis=-1, keepdims=True)
    var = jnp.mean(jnp.square(y - mu), axis=-1, keepdims=True)
    y = (y - mu) * lax.rsqrt(var + EPS)
    b_, h, n, dv = y.shape
    return y.transpose(0, 2, 1, 3).reshape(b_, n, h * dv)


def retention_chunkwise(q, k, v, log_gamma, r0):
    b_, h, n, _ = q.shape
    nc = n // RET_CHUNK
    pos = jnp.arange(RET_CHUNK, dtype=jnp.float32)
    diff = pos[:, None] - pos[None, :]
    lg = log_gamma[:, None, None]
    decay_in = jnp.where(diff >= 0, jnp.exp(jnp.maximum(diff, 0.0) * lg), 0.0)
    xi = jnp.exp((pos + 1.0) * log_gamma[:, None])[..., None]
    zeta = jnp.exp((RET_CHUNK - 1.0 - pos) * log_gamma[:, None])[..., None]
    g_chunk = jnp.exp(RET_CHUNK * log_gamma)[:, None, None]

    def to_chunks(t):
        return jnp.moveaxis(t.reshape(b_, h, nc, RET_CHUNK, t.shape[-1]), 2, 0)

    def step(r, qkv):
        qc, kc, vc = qkv
        s = jnp.einsum('bhnd,bhmd->bhnm', qc, kc) * decay_in
        o = jnp.einsum('bhnm,bhmv->bhnv', s, vc) + jnp.einsum('bhnd,bhdv->bhnv', qc, r) * xi
        r = g_chunk * r + jnp.einsum('bhmd,bhmv->bhdv', kc * zeta, vc)
        return r, o

    _, o = lax.scan(step, r0, (to_chunks(q), to_chunks(k), to_chunks(v)))
    return jnp.moveaxis(o, 0, 2).reshape(b_, h, n, -1)


def final_state(k, v, log_gamma, reverse):
    n = k.shape[2]
    m = jnp.arange(n, dtype=jnp.float32)
    expo = m if reverse else (n - 1.0 - m)
    w = jnp.exp(expo[None, :] * log_gamma[:, None])
    return jnp.einsum('bhmd,hm,bhmv->bhdv', k, w, v)


def context_states(hc, w_in_l, log_g_f, log_g_b):
    k = to_heads(hc @ w_in_l[:, K_OFF:V_OFF], RET_DK)
    v = to_heads(hc @ w_in_l[:, V_OFF:GF_OFF], RET_DV)
    return final_state(k, v, log_g_f, False), final_state(k, v, log_g_b, True)


def token_mixer(h, rope, r0_f, r0_b, w_in_l, w_four_out_l, w_ret_out_l, w_out_l, log_g_f, log_g_b):
    dt = h.dtype
    p = h @ w_in_l
    u_four, q, k, v, g_f, g_b, m_four, m_ret = jnp.split(p, SPLITS, axis=-1)
    four = fourier_mix(u_four) @ w_four_out_l
    q = to_heads(q, RET_DK) * (RET_DK ** -0.5)
    k = to_heads(k, RET_DK)
    v = to_heads(v, RET_DV)
    if rope is not None:
        q = apply_rope(q, rope[0], rope[1])
        k = apply_rope(k, rope[0], rope[1])
    y_f = retention_chunkwise(q, k, v, log_g_f, r0_f)
    y_b = jnp.flip(retention_chunkwise(jnp.flip(q, 2), jnp.flip(k, 2), jnp.flip(v, 2), log_g_b, r0_b), 2)
    ret = jax.nn.silu(g_f) * head_norm(y_f).astype(dt) + jax.nn.silu(g_b) * head_norm(y_b).astype(dt)
    ret = ret @ w_ret_out_l
    merged = jax.nn.sigmoid(m_four) * four + jax.nn.sigmoid(m_ret) * ret
    return (merged @ w_out_l).astype(dt)


def hier_moe(h, w_group_router_l, b_group_router_l, w_expert_router_l, b_expert_router_l, w_gate_l, w_up_l, w_down_l):
    t = h.shape[0]
    hf = h.astype(jnp.float32)
    g_logits = hf @ w_group_router_l.astype(jnp.float32) + b_group_router_l.astype(jnp.float32)
    g_prob = jax.nn.softmax(g_logits, axis=-1)
    grp = jnp.argmax(g_logits, axis=-1)
    g_w = jnp.take_along_axis(g_prob, grp[:, None], axis=-1)
    e_all = jnp.einsum('td,gde->tge', hf, w_expert_router_l.astype(jnp.float32)) + b_expert_router_l.astype(jnp.float32)
    e_logits = jnp.take_along_axis(e_all, grp[:, None, None], axis=1)[:, 0]
    top_v, top_i = lax.top_k(e_logits, TOP_K_IN_GROUP)
    top_w = jax.nn.softmax(top_v, axis=-1) * g_w
    expert_id = grp[:, None] * EXPERTS_PER_GROUP + top_i
    combine = jnp.sum(jax.nn.one_hot(expert_id, N_EXPERTS, dtype=jnp.float32) * top_w[..., None], axis=1)

    def expert_step(acc, xs):
        wg, wu, wd, cw = xs
        hid = jax.nn.silu(h @ wg) * (h @ wu)
        return acc + (hid * cw[:, None].astype(h.dtype)) @ wd, None

    out, _ = lax.scan(expert_step, jnp.zeros((t, h.shape[1]), h.dtype), (w_gate_l, w_up_l, w_down_l, combine.T))
    return out


def setup_inputs(seed: int = 0) -> dict:
    key = jax.random.key(seed)
    ks = jax.random.split(key, 24)

    def nrm(k, shape, scale):
        return jax.random.normal(k, shape, jnp.float32) * scale

    gam = 1.0 - 2.0 ** (-5.0 - np.arange(RET_HEADS, dtype=np.float32))
    decay_logit = jnp.asarray(np.log(gam) - np.log1p(-gam), jnp.float32)
    return {
        'x': nrm(ks[0], (BATCH, SEQ, D_MODEL), 1.0),
        'c': nrm(ks[1], (BATCH, D_MODEL), 1.0),
        'ctx': nrm(ks[2], (BATCH, CTX_LEN, D_MODEL), 1.0),
        'c_ctx': nrm(ks[3], (D_MODEL,), 1.0),
        'w_mod': nrm(ks[4], (DEPTH, D_MODEL, N_MOD * D_MODEL), 0.5 * D_MODEL ** -0.5),
        'b_mod': nrm(ks[5], (DEPTH, N_MOD * D_MODEL), 0.02),
        'norm1_g': 1.0 + nrm(ks[6], (DEPTH, D_MODEL), 0.02),
        'norm2_g': 1.0 + nrm(ks[7], (DEPTH, D_MODEL), 0.02),
        'w_in': nrm(ks[8], (DEPTH, D_MODEL, IN_WIDTH), D_MODEL ** -0.5),
        'w_four_out': nrm(ks[9], (DEPTH, FOUR_WIDTH, D_MODEL), FOUR_WIDTH ** -0.5),
        'w_ret_out': nrm(ks[10], (DEPTH, RET_V_WIDTH, D_MODEL), RET_V_WIDTH ** -0.5),
        'w_out': nrm(ks[11], (DEPTH, D_MODEL, D_MODEL), D_MODEL ** -0.5),
        'ret_decay_f': decay_logit + nrm(ks[12], (DEPTH, RET_HEADS), 0.1),
        'ret_decay_b': decay_logit + nrm(ks[13], (DEPTH, RET_HEADS), 0.1),
        'w_group_router': nrm(ks[14], (DEPTH, D_MODEL, N_GROUPS), D_MODEL ** -0.5),
        'b_group_router': nrm(ks[15], (DEPTH, N_GROUPS), 0.01),
        'w_expert_router': nrm(ks[16], (DEPTH, N_GROUPS, D_MODEL, EXPERTS_PER_GROUP), D_MODEL ** -0.5),
        'b_expert_router': nrm(ks[17], (DEPTH, N_GROUPS, EXPERTS_PER_GROUP), 0.01),
        'w_gate': nrm(ks[18], (DEPTH, N_EXPERTS, D_MODEL, EXPERT_FF), D_MODEL ** -0.5),
        'w_up': nrm(ks[19], (DEPTH, N_EXPERTS, D_MODEL, EXPERT_FF), D_MODEL ** -0.5),
        'w_down': nrm(ks[20], (DEPTH, N_EXPERTS, EXPERT_FF, D_MODEL), EXPERT_FF ** -0.5),
        'final_norm_g': 1.0 + nrm(ks[21], (D_MODEL,), 0.02),
    }


def reference(x, c, ctx, c_ctx, w_mod, b_mod, norm1_g, norm2_g, w_in, w_four_out, w_ret_out, w_out,
              ret_decay_f, ret_decay_b, w_group_router, b_group_router, w_expert_router, b_expert_router,
              w_gate, w_up, w_down, final_norm_g):
    b_, n_lat, d = x.shape
    rope = grid_rope_tables(n_lat)
    sc = jax.nn.silu(c)
    scc = jax.nn.silu(c_ctx)
    for i in range(DEPTH):
        last = i == DEPTH - 1
        log_g_f = jax.nn.log_sigmoid(ret_decay_f[i].astype(jnp.float32))
        log_g_b = jax.nn.log_sigmoid(ret_decay_b[i].astype(jnp.float32))
        mod_x = (sc @ w_mod[i] + b_mod[i])[:, None, :]
        mod_c = scc @ w_mod[i] + b_mod[i]
        sh1, s1, g1, sh2, s2, g2 = jnp.split(mod_x, N_MOD, axis=-1)
        csh1, cs1, cg1, csh2, cs2, cg2 = jnp.split(mod_c, N_MOD, axis=-1)
        hx = modulate(rmsnorm(x, norm1_g[i]), sh1, s1)
        hc = modulate(rmsnorm(ctx, norm1_g[i]), csh1, cs1)
        r_f, r_b = context_states(hc, w_in[i], log_g_f, log_g_b)
        x = x + g1 * token_mixer(hx, rope, r_f, r_b, w_in[i], w_four_out[i], w_ret_out[i], w_out[i], log_g_f, log_g_b)
        if not last:
            zero_state = jnp.zeros_like(r_f)
            ctx = ctx + cg1 * token_mixer(hc, None, zero_state, zero_state, w_in[i], w_four_out[i], w_ret_out[i], w_out[i], log_g_f, log_g_b)
        hx2 = modulate(rmsnorm(x, norm2_g[i]), sh2, s2)
        x = x + g2 * hier_moe(hx2.reshape(-1, d), w_group_router[i], b_group_router[i], w_expert_router[i],
                              b_expert_router[i], w_gate[i], w_up[i], w_down[i]).reshape(x.shape)
        if not last:
            hc2 = modulate(rmsnorm(ctx, norm2_g[i]), csh2, cs2)
            ctx = ctx + cg2 * hier_moe(hc2.reshape(-1, d), w_group_router[i], b_group_router[i], w_expert_router[i],
                                       b_expert_router[i], w_gate[i], w_up[i], w_down[i]).reshape(ctx.shape)
    return rmsnorm(x, final_norm_g)
```

```python
import math
from contextlib import ExitStack
import numpy as np
import ml_dtypes
import concourse.bass as bass
import concourse.mybir as mybir
from concourse.bass_utils import run_bass_kernel_spmd

F32 = mybir.dt.float32
BF16 = mybir.dt.bfloat16
ALU = mybir.AluOpType
AF = mybir.ActivationFunctionType
AX = mybir.AxisListType

ENGS = ("pe", "act", "dve", "pool", "sp")
SAME_ENGINE_SYNC = {"act", "dve", "pool"}


class Sched:
    def __init__(self, nc):
        self.nc = nc
        self.prog = {e: [] for e in ENGS}
        self.cnt = {e: 0 for e in ENGS}
        self.clock = {e: {} for e in ENGS}
        self.res = {}
        self.chan_cnt = {}
        self.semkeys = list(ENGS)

    def _need(self, eng, tok, waits, same_ok):
        if tok is None:
            return
        key, val, src = tok
        if src == "dma":
            val = self.chan_cnt[key]
        if src == eng and key == eng and eng not in SAME_ENGINE_SYNC:
            return
        if self.clock[eng].get(key, 0) >= val:
            return
        waits[key] = max(waits.get(key, 0), val)

    def op(self, eng, fn, reads=(), writes=(), chan=None, inc=True):
        waits = {}
        for r in reads:
            st = self.res.get(r)
            if st:
                self._need(eng, st[0], waits, False)
        for w in writes:
            st = self.res.get(w)
            if st:
                self._need(eng, st[0], waits, False)
                for t in st[1]:
                    self._need(eng, t, waits, True)
        for k, v in waits.items():
            self.clock[eng][k] = v
        if chan is None and not inc:
            assert eng == "pe"
            tok = (eng, self.cnt[eng] + 1, eng)
            inc = None
        elif chan is None:
            self.cnt[eng] += 1
            tok = (eng, self.cnt[eng], eng)
            inc = (eng, 1)
        else:
            if chan not in self.chan_cnt:
                self.chan_cnt[chan] = 0
                self.semkeys.append(chan)
            self.chan_cnt[chan] += 16
            tok = (chan, self.chan_cnt[chan], "dma")
            inc = (chan, 16)
        self.prog[eng].append((sorted(waits.items(), key=str), fn, inc))
        for r in reads:
            self.res.setdefault(r, [None, []])[1].append(tok)
        for w in writes:
            self.res[w] = [tok, []]
        return tok

    def barrier(self):
        tot = {e: self.cnt[e] for e in ENGS}
        tot.update(self.chan_cnt)
        for e in ENGS:
            waits = {}
            for k, v in tot.items():
                if v > 0 and self.clock[e].get(k, 0) < v and not (k == e and e in ("pe", "sp")):
                    waits[k] = v
                    self.clock[e][k] = v
            if waits:
                self.prog[e].append((sorted(waits.items(), key=str), None, None))
        self.res = {}

    def emit(self):
        nc = self.nc
        with ExitStack() as es:
            sems = {k: es.enter_context(nc.semaphore("s_" + str(k))) for k in self.semkeys}
            block = es.enter_context(nc.Block())

            def run(name):
                def body(e):
                    for waits, fn, inc in self.prog[name]:
                        for k, v in waits:
                            e.wait_ge(sems[k], v)
                        if fn is not None:
                            ins = fn(e)
                            if inc is not None:
                                ins.then_inc(sems[inc[0]], inc[1])
                return body

            block.tensor(run("pe"))
            block.scalar(run("act"))
            block.vector(run("dve"))
            block.gpsimd(run("pool"))
            block.sync(run("sp"))


D = 2048
NJ = 16
FW = 1024
NH = 8
DK = 128
DV = 256
Q_OFF = FW
K_OFF = Q_OFF + NH * DK
V_OFF = K_OFF + NH * DK
GF_OFF = V_OFF + NH * DV
GB_OFF = GF_OFF + NH * DV
MF_OFF = GB_OFF + NH * DV
MR_OFF = MF_OFF + D
IN_W = MR_OFF + D
FF = 512
EPS = 1e-6
GRID_W = 64
ROPE_BASE = 10000.0


class Cfg:
    def __init__(self, seq=2048, ctx=256, ngroups=4, batch=4):
        self.SEQ, self.CTX, self.NG, self.B = seq, ctx, ngroups, batch
        self.TOK = seq // 2
        self.NT = self.TOK // 128
        self.NC = ctx // 128
        self.NE = ngroups * 8
        self.NR = ngroups + self.NE
        self.TB = [(t0, min(512, self.TOK - t0)) for t0 in range(0, self.TOK, 512)]


def host_tables(cfg, half):
    TOK, SEQ, NT = cfg.TOK, cfg.SEQ, cfg.NT
    own = np.arange(half * TOK, (half + 1) * TOK)
    oth = np.arange((1 - half) * TOK, (2 - half) * TOK)
    order = np.concatenate([own, oth])
    ang = 2.0 * np.pi * ((order[:, None].astype(np.int64) * own[None, :].astype(np.int64)) % SEQ) / SEQ
    dft = np.stack([np.cos(ang), np.sin(ang)], axis=1).astype(ml_dtypes.bfloat16)
    f = np.arange(128)
    angg = 2.0 * np.pi * ((f[:, None] * f[None, :]) % 128) / 128.0
    n_freq = DK // 4
    inv = ROPE_BASE ** (-np.arange(n_freq, dtype=np.float32) / n_freq)

    def rope(pos):
        row = (pos // GRID_W).astype(np.float32)
        col = (pos % GRID_W).astype(np.float32)
        a = np.concatenate([row[:, None] * inv, col[:, None] * inv], axis=-1).astype(np.float32)
        return np.cos(a), np.sin(a)

    co, so = rope(own)
    NTT = 2 * NT
    CBW = 640 + 2 * TOK + NTT * 64 + NTT * 128
    cb = np.zeros((128, CBW), ml_dtypes.bfloat16)
    cb[:, 0:128] = np.eye(128)
    psw = np.zeros((128, 128), np.float32)
    for m in range(128):
        psw[(m + 64) % 128, m] = 1.0
    cb[:, 128:256] = psw
    cb[:, 256:384] = np.cos(angg)
    cb[:, 384:512] = -np.sin(angg)
    cb[:, 512:640] = 0
    cosT = np.concatenate([co.T, co.T], axis=0)
    sinS = np.concatenate([-so.T, so.T], axis=0)
    cb[:, 640:640 + TOK] = cosT
    cb[:, 640 + TOK:640 + 2 * TOK] = sinS
    call, sall = rope(order)
    cc = call.reshape(NTT, 128, 64).transpose(1, 0, 2)
    ss = np.concatenate([-sall, sall], axis=1).reshape(NTT, 128, 128).transpose(1, 0, 2)
    o_ = 640 + 2 * TOK
    cb[:, o_:o_ + NTT * 64] = cc.reshape(128, -1)
    cb[:, o_ + NTT * 64:o_ + NTT * 64 + NTT * 128] = ss.reshape(128, -1)
    j = np.arange(128)[:, None].astype(np.float32)
    i = np.arange(128)[None, :].astype(np.float32)
    s = DK ** -0.5
    dmat = (i - j) * np.ones((128, 128), np.float32)
    mf = (i >= j).astype(np.float32) * s
    mb = (j >= i).astype(np.float32) * s
    pr1 = (i + 1.0) * np.ones((128, 1), np.float32)
    prb = (128.0 - i) * np.ones((128, 1), np.float32)
    m = np.arange(128, dtype=np.float32)
    MT = max(cfg.NC, NT)
    pc = np.zeros((128, 3, MT), np.float32)
    for t in range(MT):
        pc[:, 0, t] = cfg.CTX - 1 - (128 * t + m)
        pc[:, 1, t] = 128 * t + m
        pc[:, 2, t] = TOK - 1 - (128 * t + m)
    pz = np.stack([127.0 - m, m], axis=1)
    fl = np.zeros((128, 16), np.float32)
    fl[:, 0:8] = 1.0 if half == 1 else 0.0
    fl[:, 8:16] = 1.0 if half == 0 else 0.0
    cf = np.concatenate([dmat, mf, mb, pr1, prb,
                         pc.reshape(128, -1), pz, fl], axis=1).astype(np.float32)
    return dft, cb, np.ascontiguousarray(cf)


def build(cfg, debug=None):
    TOK, NT, NC, NE, NG, NR = cfg.TOK, cfg.NT, cfg.NC, cfg.NE, cfg.NG, cfg.NR
    NTT = 2 * NT
    MT = max(NC, NT)
    nc = bass.Bass("TRN2", target_bir_lowering=False)

    def din(name, shape, dt=F32):
        return nc.dram_tensor(name, list(shape), dt, kind="ExternalInput").ap()

    x_own = din("x_own", [TOK, D]); x_oth = din("x_oth", [TOK, D]); ctx_d = din("ctx", [cfg.CTX, D])
    cvec = din("cvec", [D]); cctx = din("c_ctx", [D])
    w_mod = din("w_mod", [D, 6 * D]); b_mod = din("b_mod", [6 * D])
    n1g = din("norm1_g", [D]); n2g = din("norm2_g", [D]); fng = din("final_norm_g", [D])
    w_in = din("w_in", [D, IN_W]); w4 = din("w_four_out", [FW, D]); wro = din("w_ret_out", [D, D]); wout = din("w_out", [D, D])
    decay = din("decay", [16])
    w_rt = din("w_router", [D, NR]); b_rt = din("b_router", [NR])
    wg_d = din("w_gate", [NE, D, FF]); wu_d = din("w_up", [NE, D, FF]); wd_d = din("w_down", [NE, FF, D])
    dft_d = din("dft", [cfg.SEQ, 2, TOK], BF16)
    CBW = 640 + 2 * TOK + NTT * 64 + NTT * 128
    cb_d = din("cbf", [128, CBW], BF16)
    CFW = 5 * 128 + 3 * MT + 2 + 16
    cf_d = din("cf32", [128, CFW])
    out_d = nc.dram_tensor("out", [TOK, D], F32, kind="ExternalOutput").ap()
    mscr = nc.dram_tensor("mscr", [NJ, 128, TOK], BF16, kind="Internal").ap()
    rscr = nc.dram_tensor("rscr", [NJ, 128, TOK], BF16, kind=("ExternalOutput" if (debug and debug[0] == "stop_m3") else "Internal")).ap()
    dbg_d = None
    if debug:
        dbg_d = nc.dram_tensor("dbg", list(debug[1]), F32, kind="ExternalOutput").ap()

    es = ExitStack()
    ARENA = 207 * 512
    arena = es.enter_context(nc.sbuf_tensor("arena", [128, ARENA], BF16))
    PS = [es.enter_context(nc.psum_tensor("ps%d" % i, [128, 512], F32)) for i in range(6)]
    PSB = es.enter_context(nc.psum_tensor("psb", [128, 2048], BF16))
    PSBF = [PSB[:, 0:1024].bitcast(F32), PSB[:, 1024:2048].bitcast(F32)]
    S = Sched(nc)

    class Reg:
        def __init__(self, base, size):
            self.base, self.size, self.off = base, size, 0

        def reset(self):
            self.off = 0

        def get(self, nelem, dt, shape=None):
            n2 = nelem * (2 if dt == F32 else 1)
            if n2 % 2:
                n2 += 1
            assert self.off + n2 <= self.size, ("region overflow", self.off, n2, self.size)
            a = self.base + self.off
            self.off += n2
            ap = arena[:, a:a + n2]
            if dt == F32:
                ap = ap.bitcast(F32)
            if shape is not None:
                if len(shape) == 2:
                    ap = ap.rearrange("p (a b) -> p a b", a=shape[0])
                else:
                    ap = ap.rearrange("p (a b c) -> p a b c", a=shape[0], b=shape[1])
            return ap

    K = 512
    pos = [0]

    def region(kb):
        r = Reg(pos[0], int(kb * K))
        pos[0] += int(kb * K)
        assert pos[0] <= ARENA, pos[0]
        return r

    R_K = region(36)
    R_H = region(64)
    R_C = region(8)
    R_W = region(48)
    R_S = region(51)

    cb = R_K.get(CBW, BF16)
    ident = cb[:, 0:128]; pswap = cb[:, 128:256]; cgm = cb[:, 256:384]; nsg = cb[:, 384:512]
    cosT = cb[:, 640:640 + TOK]; sinS = cb[:, 640 + TOK:640 + 2 * TOK]
    cf = R_K.get(CFW, F32)
    o = 640 + 2 * TOK
    cos_tm = cb[:, o:o + NTT * 64].rearrange("p (t f) -> p t f", t=NTT); o += NTT * 64
    ss_tm = cb[:, o:o + NTT * 128].rearrange("p (t f) -> p t f", t=NTT)
    o = 0
    dmat = cf[:, o:o + 128]; o += 128
    mfm = cf[:, o:o + 128]; o += 128
    mbm = cf[:, o:o + 128]; o += 128
    pr1 = cf[:, o:o + 128]; o += 128
    prb = cf[:, o:o + 128]; o += 128
    pcs = cf[:, o:o + 3 * MT].rearrange("p (a t) -> p a t", a=3); o += 3 * MT
    pz = cf[:, o:o + 2]; o += 2
    fl16 = cf[:, o:o + 16]; o += 16
    Dm = R_K.get(16 * 128, BF16, (16, 128))
    Xi = R_K.get(16 * 128, BF16, (16, 128))
    DEAD_END = R_K.off
    lg = R_K.get(16, F32); nlg = R_K.get(16, F32)
    G128 = R_K.get(16, F32)
    Zt = R_K.get(16, F32)
    Wc = R_K.get(NC * 16, F32, (NC, 16))
    Wo = R_K.get(NT * 16, F32, (NT, 16))
    A1 = R_K.get(16, F32); B1 = R_K.get(16, F32); A1c = R_K.get(16, F32); B1c = R_K.get(16, F32)
    A2 = R_K.get(16, F32); B2 = R_K.get(16, F32)
    g1b = R_K.get(D, F32)
    g2b = R_K.get(D, BF16)
    ones1 = R_K.get(2, F32)
    small = R_K.get(64, F32)

    def dma(eng, out, in_, reads, writes, chan):
        return S.op(eng, lambda e: e.dma_start(out=out, in_=in_), reads=reads, writes=writes, chan=chan)

    def mm(out, lhsT, rhs, start, stop, reads, writes, sig=False):
        return S.op("pe", lambda e: e.matmul(out, lhsT=lhsT, rhs=rhs, start=start, stop=stop), reads=reads, writes=writes,
                    inc=bool(stop or sig))

    def act(out, in_, func, reads, writes, **kw):
        return S.op("act", lambda e: e.activation(out=out, in_=in_, func=func, **kw), reads=reads, writes=writes)

    def tt(eng, out, in0, in1, op, reads, writes):
        return S.op(eng, lambda e: e.tensor_tensor(out=out, in0=in0, in1=in1, op=op), reads=reads, writes=writes)

    def ts(eng, out, in0, s1, s2, op0, op1, reads, writes):
        if s2 is None:
            return S.op(eng, lambda e: e.tensor_scalar(out=out, in0=in0, scalar1=s1, scalar2=None, op0=op0), reads=reads, writes=writes)
        return S.op(eng, lambda e: e.tensor_scalar(out=out, in0=in0, scalar1=s1, scalar2=s2, op0=op0, op1=op1), reads=reads, writes=writes)

    def cp(eng, out, in_, reads, writes):
        if eng == "act":
            return S.op("act", lambda e: e.copy(out=out, in_=in_), reads=reads, writes=writes)
        return S.op(eng, lambda e: e.tensor_copy(out=out, in_=in_), reads=reads, writes=writes)

    ring = [R_W.get(16 * K // 1, BF16) for _ in range(3)] if False else None
    ringb = [arena[:, R_W.base + i * 16 * K:R_W.base + (i + 1) * 16 * K] for i in range(3)]
    ring_i = [0]

    def ring_next():
        i = ring_i[0] % 3
        ring_i[0] += 1
        return i

    def wload(slot, off_el, dram_ap, shape3, rname):
        a, b = shape3
        ap = ringb[slot][:, off_el:off_el + a * b].rearrange("p (a b) -> p a b", a=a)
        step = max(1, min(a, 4096 // max(1, 1)))
        step = max(1, a // 4) if a >= 4 else a
        for a0 in range(0, a, step):
            a1 = min(a, a0 + step)
            dma("pool", ap[:, a0:a1, :], dram_ap[:, a0:a1, :], [], [rname], "ring%d" % slot)
        return ap

    def wload_into(dst3, dram_ap, rname, slot):
        a = dst3.shape[1]
        step = max(1, a // 4) if a >= 4 else a
        for a0 in range(0, a, step):
            a1 = min(a, a0 + step)
            dma("pool", dst3[:, a0:a1, :], dram_ap[:, a0:a1, :], [], [rname], "ring%d" % slot)

    dma("sp", cb, cb_d, [], ["cb"], "c0")
    dma("sp", cf, cf_d, [], ["cf"], "c0")
    dec = small[:, 0:16]
    dma("sp", dec, decay.partition_broadcast(128), [], ["dec"], "c0")
    gn1 = small[:, 16:32]; gn2 = small[:, 32:48]
    dma("sp", gn1, n1g.rearrange("(p j) -> p j", j=16), [], ["gn1"], "c0")
    dma("sp", gn2, n2g.rearrange("(p j) -> p j", j=16), [], ["gn2"], "c0")
    S.op("dve", lambda e: e.memset(ones1, 1.0), writes=["ones1"])

    R_S.reset()
    tA = R_S.get(16, F32); tB = R_S.get(16, F32); tC = R_S.get(16, F32); tD = R_S.get(16, F32)
    ts("dve", tB, dec, -1.0, None, ALU.mult, None, ["dec"], ["tB"])
    tt("dve", tA, dec, tB, ALU.max, ["dec", "tB"], ["tA"])
    act(tA, tA, AF.Exp, ["tA"], ["tA"], scale=-1.0)
    ts("dve", tB, tA, 2.0, None, ALU.add, None, ["tA"], ["tB"])
    S.op("dve", lambda e: e.reciprocal(out=tB, in_=tB), reads=["tB"], writes=["tB"])
    tt("dve", tB, tA, tB, ALU.mult, ["tA", "tB"], ["tB"])
    tt("dve", tC, tB, tB, ALU.mult, ["tB"], ["tC"])
    ts("dve", tD, tC, 1.0 / 9.0, 1.0 / 7.0, ALU.mult, ALU.add, ["tC"], ["tD"])
    for cst in (1.0 / 5.0, 1.0 / 3.0, 1.0):
        tt("dve", tD, tD, tC, ALU.mult, ["tD", "tC"], ["tD"])
        ts("dve", tD, tD, cst, None, ALU.add, None, ["tD"], ["tD"])
    tt("dve", tD, tD, tB, ALU.mult, ["tD", "tB"], ["tD"])
    ts("dve", tA, dec, 0.0, None, ALU.min, None, ["dec"], ["tA"])
    S.op("dve", lambda e: e.scalar_tensor_tensor(out=lg, in0=tD, scalar=-2.0, in1=tA, op0=ALU.mult, op1=ALU.add),
         reads=["tD", "tA"], writes=["lg"])
    ts("dve", nlg, lg, -1.0, None, ALU.mult, None, ["lg"], ["nlg"])
    act(G128, lg, AF.Exp, ["lg"], ["G128"], scale=128.0)
    GT = tA
    act(GT, lg, AF.Exp, ["lg"], ["GT"], scale=float(TOK))
    mulc = tB
    ts("dve", mulc, GT, -1.0, None, ALU.add, None, ["GT"], ["mulc"])
    tt("dve", mulc, mulc, fl16, ALU.mult, ["mulc", "cf"], ["mulc"])
    ts("dve", mulc, mulc, 1.0, None, ALU.add, None, ["mulc"], ["mulc"])
    for d_ in range(2):
        cs = slice(d_ * 8, d_ * 8 + 8)
        act(Zt[:, cs], lg[:, cs], AF.Exp, ["lg", "cf"], ["Zt"], scale=pz[:, d_:d_ + 1])
        for t in range(NC):
            act(Wc[:, t, cs], lg[:, cs], AF.Exp, ["lg", "cf"], ["Wc"], scale=pcs[:, (0 if d_ == 0 else 1), t:t + 1])
            tt("dve", Wc[:, t, cs], Wc[:, t, cs], mulc[:, cs], ALU.mult, ["Wc", "mulc"], ["Wc"])
        for t in range(NT):
            act(Wo[:, t, cs], lg[:, cs], AF.Exp, ["lg", "cf"], ["Wo"], scale=pcs[:, (2 if d_ == 0 else 1), t:t + 1])
            tt("dve", Wo[:, t, cs], Wo[:, t, cs], fl16[:, cs], ALU.mult, ["Wo", "cf"], ["Wo"])
    mtmp = R_S.get(128, F32)
    for q in range(16):
        if q < 8:
            act(mtmp, dmat, AF.Exp, ["lg", "cf", "mtmp"], ["mtmp"], scale=lg[:, q:q + 1])
            tt("dve", Dm[:, q, :], mtmp, mfm, ALU.mult, ["mtmp", "cf"], ["Dm"])
            act(mtmp, pr1, AF.Exp, ["lg", "cf", "mtmp"], ["mtmp"], scale=lg[:, q:q + 1])
        else:
            act(mtmp, dmat, AF.Exp, ["nlg", "cf", "mtmp"], ["mtmp"], scale=nlg[:, q:q + 1])
            tt("dve", Dm[:, q, :], mtmp, mbm, ALU.mult, ["mtmp", "cf"], ["Dm"])
            act(mtmp, prb, AF.Exp, ["lg", "cf", "mtmp"], ["mtmp"], scale=lg[:, q:q + 1])
        ts("dve", Xi[:, q, :], mtmp, DK ** -0.5, None, ALU.mult, None, ["mtmp"], ["Xi"])

    vbx = arena[:, R_H.base:R_H.base + 2 * 6 * D].bitcast(F32)
    vbc = arena[:, R_H.base + 2 * 6 * D:R_H.base + 2 * 8 * D].bitcast(F32)
    craw = R_S.get(32, F32)
    dma("sp", craw[:, 0:16], cvec.rearrange("(p j) -> p j", j=16), [], ["craw"], "c1")
    dma("sp", craw[:, 16:32], cctx.rearrange("(p j) -> p j", j=16), [], ["craw"], "c1")
    csil = R_S.get(32, BF16)
    act(csil, craw, AF.Silu, ["craw"], ["csil"])
    screp = R_S.get(32 * 128, BF16, (32, 128))
    cp("dve", screp, csil.unsqueeze(2).to_broadcast([128, 32, 128]), ["csil"], ["screp"])
    bmb = R_S.get(512, F32)
    NB = 6 * D // 512
    for blk in range(NB):
        sl = ring_next()
        wb = wload(sl, 0, w_mod[:, blk * 512:(blk + 1) * 512].rearrange("(p j) f -> p j f", j=16), (16, 512), ("ring", sl))
        dma("sp", bmb, b_mod[blk * 512:(blk + 1) * 512].partition_broadcast(128), [], ["bmb"], "c2")
        for which in range(2 if blk < 8 else 1):
            pb = blk % 2 + 2 * which
            for j in range(16):
                mm(PS[pb][:, :], screp[:, which * 16 + j, :], wb[:, j, :], j == 0, j == 15, ["screp", ("ring", sl)], [("ps", pb)])
            dst = (vbx if which == 0 else vbc)[:, blk * 512:(blk + 1) * 512]
            tt("dve", dst, PS[pb][:, :], bmb, ALU.add, [("ps", pb), "bmb"], ["vb"])
    colv = R_S.get(6 * 16, F32, (6, 16))
    srcs = [(vbx, 0), (vbx, D), (vbc, 0), (vbc, D), (vbx, 3 * D), (vbx, 4 * D)]
    for vi, (vsrc, base) in enumerate(srcs):
        for j in range(16):
            mm(PS[4][:, vi * 16 + j:vi * 16 + j + 1], vsrc[0:1, base + j:base + D:16], ones1[0:1, 0:1], True, True,
               ["vb", "ones1"], [("ps", 4)])
    cp("dve", colv.rearrange("p a b -> p (a b)"), PS[4][:, 0:96], [("ps", 4)], ["colv"])
    for (Adst, Bdst, sidx, shidx, gsrc, gname) in ((A1, B1, 1, 0, gn1, "gn1"), (A1c, B1c, 3, 2, gn1, "gn1"), (A2, B2, 5, 4, gn2, "gn2")):
        S.op("dve", lambda e, Adst=Adst, sidx=sidx, gsrc=gsrc: e.scalar_tensor_tensor(
            out=Adst, in0=colv[:, sidx, :], scalar=1.0, in1=gsrc, op0=ALU.add, op1=ALU.mult),
            reads=["colv", gname], writes=["AB"])
        cp("dve", Bdst, colv[:, shidx, :], ["colv"], ["AB"])
    cp("dve", g1b, vbx[:, 2 * D:3 * D], ["vb"], ["g1b"])
    cp("dve", g2b, vbx[:, 5 * D:6 * D], ["vb"], ["g2b"])
    S.barrier()

    hTo = arena[:, R_H.base:R_H.base + 16 * TOK].rearrange("p (j t) -> p j t", j=16)
    hTw = arena[:, R_H.base + 16 * TOK:R_H.base + 32 * TOK].rearrange("p (j t) -> p j t", j=16)
    hcT = arena[:, R_C.base:R_C.base + 16 * cfg.CTX].rearrange("p (j t) -> p j t", j=16)
    R_S.reset()
    xst = [R_S.get(D, F32) for _ in range(2)]
    xsb = [R_S.get(D, BF16) for _ in range(2)]
    junk = R_S.get(D, BF16)
    ssq = R_S.get(2 * NT + NC, F32)
    pre_m2_sl = ring_next()
    pre_m2_w = wload(pre_m2_sl, 0, w_in[:, 0:256].rearrange("(p j) f -> p j f", j=16), (16, 256), ("ring", pre_m2_sl))
    tiles = [(x_own, t, hTw, A1, B1) for t in range(NT)] + [(x_oth, t, hTo, A1, B1) for t in range(NT)] + \
            [(ctx_d, t, hcT, A1c, B1c) for t in range(NC)]
    for ti, (src, t, dstT, Av, Bv) in enumerate(tiles):
        b = ti % 2
        dma("sp", xst[b], src[t * 128:(t + 1) * 128, :], [], [("xst", b)], "xs%d" % b)
        sq = ssq[:, ti:ti + 1]
        act(junk, xst[b], AF.Square, [("xst", b)], ["junk", ("ssq", ti)], accum_out=sq)
        act(sq, sq, AF.Ln, [("ssq", ti)], [("ssq", ti)], scale=1.0 / D, bias=EPS)
        act(sq, sq, AF.Exp, [("ssq", ti)], [("ssq", ti)], scale=-0.5)
        act(xsb[b], xst[b], AF.Copy, [("xst", b), ("ssq", ti)], [("xsb", b)], scale=sq)
        for j in range(16):
            S.op("pe", lambda e, j=j, b=b: e.transpose(out=PSB[:, j * 128:(j + 1) * 128], in_=xsb[b][:, j::16], identity=ident),
                 reads=[("xsb", b), "cb"], writes=[("psb", j // 8)])
        for j in range(16):
            ts("dve", dstT[:, j, t * 128:(t + 1) * 128], PSB[:, j * 128:(j + 1) * 128], Av[:, j:j + 1], Bv[:, j:j + 1],
               ALU.mult, ALU.add, [("psb", j // 8), "AB"], ["hT"])
    S.barrier()

    def dbg_dump(ap_list):
        for a, d_ in ap_list:
            dma("sp", d_, a, [], ["dbg"], "dbg")
        S.barrier()
        S.emit()
        es.close()
        return nc

    if debug and debug[0] == "hT":
        R_S.reset()
        tmp = R_S.get(16 * 128, F32, (16, 128))
        cp("dve", tmp, hTw[:, :, 0:128], [], ["tmp"])
        S.barrier()
        tmp2 = R_S.get(16 * 128, F32, (16, 128))
        cp("dve", tmp2, hcT[:, :, 0:128], [], ["tmp2"])
        S.barrier()
        return dbg_dump([(tmp, dbg_d[0]), (tmp2, dbg_d[1])])

    inv_scale = 1.0 / math.sqrt(cfg.SEQ * 128.0)
    R_S.reset()
    u_gp = R_S.get(NTT * 256, BF16, (NTT, 256))
    tbl = [R_S.get(2 * TOK, BF16, (2, TOK)) for _ in range(2)]
    PT = R_S.get(4 * TOK, BF16, (4, TOK))
    YT = R_S.get(8 * TOK, BF16, (8, TOK))
    ptmp = [R_S.get(512, F32) for _ in range(2)]
    hsrc = [(hTw, t) for t in range(NT)] + [(hTo, t) for t in range(NT)]
    tbl_i = 0
    for gp in range(4):
        if gp == 0:
            sl, wu = pre_m2_sl, pre_m2_w
        else:
            sl = ring_next()
            wu = wload(sl, 0, w_in[:, gp * 256:(gp + 1) * 256].rearrange("(p j) f -> p j f", j=16), (16, 256), ("ring", sl))
        for ti, (hsrcT, t) in enumerate(hsrc):
            pb = ti % 2
            for j in range(16):
                mm(PS[pb][:, 0:256], hsrcT[:, j, t * 128:(t + 1) * 128], wu[:, j, :], j == 0, j == 15, ["hT", ("ring", sl)], [("ps", pb)])
            cp("act" if ti % 2 else "dve", u_gp[:, ti, :], PS[pb][:, 0:256], [("ps", pb)], [("u_gp", ti)])
        accs = [(g2, cs, bi) for g2 in range(2) for cs in range(2) for bi in range(len(cfg.TB))]
        assert len(accs) <= 8

        def accbank(ai, tsz):
            if ai < 6:
                return PS[ai][:, 0:tsz], ("ps", ai)
            return PSBF[ai - 6][:, 0:tsz], ("psb", ai - 6)
        for ncx in range(NTT):
            tb_ = tbl_i % 2
            tbl_i += 1
            dma("sp", tbl[tb_], dft_d[ncx * 128:(ncx + 1) * 128, :, :], [], [("tbl", tb_)], "tbl%d" % tb_)
            for ai, (g2, cs, bi) in enumerate(accs):
                t0, tsz = cfg.TB[bi]
                outp, rn = accbank(ai, tsz)
                mm(outp, u_gp[:, ncx, g2 * 128:(g2 + 1) * 128], tbl[tb_][:, cs, t0:t0 + tsz], ncx == 0, ncx == NTT - 1,
                   [("u_gp", ncx), ("tbl", tb_)], [rn], sig=True)
        for ai, (g2, cs, bi) in enumerate(accs):
            t0, tsz = cfg.TB[bi]
            outp, rn = accbank(ai, tsz)
            cp("act" if ai % 2 else "dve", PT[:, g2 * 2 + cs, t0:t0 + tsz], outp, [rn], ["PT"])
        for g2 in range(2):
            g = gp * 2 + g2
            for bi, (t0, tsz) in enumerate(cfg.TB):
                pb = 4 + (g2 + bi) % 2
                mm(PS[pb][:, 0:tsz], cgm, PT[:, g2 * 2 + 0, t0:t0 + tsz], True, False, ["cb", "PT"], [("ps", pb)])
                mm(PS[pb][:, 0:tsz], nsg, PT[:, g2 * 2 + 1, t0:t0 + tsz], False, True, ["cb", "PT"], [("ps", pb)])
                act(YT[:, g, t0:t0 + tsz], PS[pb][:, 0:tsz], AF.Copy, [("ps", pb)], ["YT"], scale=inv_scale)
    mst = [R_S.get(512, BF16) for _ in range(2)]
    mi = 0
    for ob in range(4):
        sl4 = ring_next()
        w4b = wload(sl4, 0, w4[:, ob * 512:(ob + 1) * 512].rearrange("(g p) f -> p g f", p=128), (8, 512), ("ring", sl4))
        slm = ring_next()
        wmf = wload(slm, 0, w_in[:, MF_OFF + ob * 512:MF_OFF + (ob + 1) * 512].rearrange("(p j) f -> p j f", j=16), (16, 512), ("ring", slm))
        for o4 in range(4):
            oc = ob * 4 + o4
            for bi, (t0, tsz) in enumerate(cfg.TB):
                pa, pb = (mi % 2) * 2, (mi % 2) * 2 + 1
                for g in range(8):
                    mm(PS[pa][:, 0:tsz], w4b[:, g, o4 * 128:(o4 + 1) * 128], YT[:, g, t0:t0 + tsz], g == 0, g == 7, [("ring", sl4), "YT"], [("ps", pa)])
                for j in range(16):
                    mm(PS[pb][:, 0:tsz], wmf[:, j, o4 * 128:(o4 + 1) * 128], hTw[:, j, t0:t0 + tsz], j == 0, j == 15, [("ring", slm), "hT"], [("ps", pb)])
                k2 = mi % 2
                act(ptmp[k2][:, 0:tsz], PS[pb][:, 0:tsz], AF.Sigmoid, [("ps", pb)], [("ptmp", k2)])
                tt("dve", mst[k2][:, 0:tsz], PS[pa][:, 0:tsz], ptmp[k2][:, 0:tsz], ALU.mult, [("ps", pa), ("ptmp", k2)], [("mst", k2)])
                dma("sp", mscr[oc, :, t0:t0 + tsz], mst[k2][:, 0:tsz], [("mst", k2)], ["mscr"], "mst%d" % k2)
                mi += 1
    S.barrier()

    if debug and debug[0] == "stop_m2":
        R_S.reset()
        tmpd = R_S.get(TOK, F32)
        cp("dve", tmpd, YT[:, 0, :], [], ["tmpd"])
        S.barrier()
        dma("sp", dbg_d, tmpd, [], ["dbg"], "dbg")
        S.barrier(); S.emit(); es.close()
        return nc

    R_S.reset()
    _alias0 = R_S.off
    qraw = R_S.get(512, BF16); kraw = R_S.get(512, BF16)
    rt1 = R_S.get(512, F32); rt2 = R_S.get(512, F32)
    qrot = R_S.get(TOK, BF16); krot = R_S.get(TOK, BF16)
    qxf = R_S.get(TOK, BF16); qxb = R_S.get(TOK, BF16)
    kzf = R_S.get(NT * 128, BF16, (NT, 128)); kzb = R_S.get(NT * 128, BF16, (NT, 128))
    vown = R_S.get(NT * 256, BF16, (NT, 256))
    SfT = R_S.get(NT * 128, BF16, (NT, 128)); SbT = R_S.get(NT * 128, BF16, (NT, 128))
    ybf = R_S.get(NT * 256, BF16, (NT, 256)); ybb = R_S.get(NT * 256, BF16, (NT, 256))
    sgf = R_S.get(NT * 256, BF16, (NT, 256)); sgb = R_S.get(NT * 256, BF16, (NT, 256))
    R32 = [R_S.get(256, F32) for _ in range(2)]
    R16 = [R_S.get(256, BF16) for _ in range(2)]
    krt = R_S.get(128, F32); kr2 = R_S.get(128, F32)
    kwt = [R_S.get(128, BF16) for _ in range(2)]
    vbt = R_S.get(256, BF16)
    st6 = R_S.get(2 * NT * 6, F32, (2 * NT, 6))
    mv = R_S.get(2 * NT * 2, F32, (2 * NT, 2))
    rsd = R_S.get(2 * NT, F32); nmr = R_S.get(2 * NT, F32)
    zt = [R_S.get(256, BF16) for _ in range(2)]
    kb16 = R_S.get(128, BF16); sw16 = R_S.get(512, BF16); st16 = R_S.get(128, BF16)
    assert 2 * TOK <= 3072
    retT = arena[:, R_S.base + _alias0:R_S.base + _alias0 + 2 * TOK].rearrange("p (a b) -> p a b", a=2)
    RT_AL = ["retT", "rawq", "rawk", "rt1", "rt2"]

    lim = debug[2] if (debug and debug[0] == "stop_m3" and len(debug) > 2) else 99
    import os as _os
    for h in range(NH):
        slA = ring_next()
        wq = wload(slA, 0, w_in[:, Q_OFF + h * 128:Q_OFF + (h + 1) * 128].rearrange("(p j) f -> p j f", j=16), (16, 128), ("ring", slA))
        wkv = ringb[slA][:, 16 * 128:16 * 128 + 16 * 384].rearrange("p (a b) -> p a b", a=16)
        wk = wkv[:, :, 0:128]
        wload_into(wk, w_in[:, K_OFF + h * 128:K_OFF + (h + 1) * 128].rearrange("(p j) f -> p j f", j=16), ("ring", slA), slA)
        wload_into(wkv[:, :, 128:384], w_in[:, V_OFF + h * 256:V_OFF + (h + 1) * 256].rearrange("(p j) f -> p j f", j=16), ("ring", slA), slA)
        slB = ring_next()
        wg2 = ringb[slB][:, 0:16 * 512].rearrange("p (a b) -> p a b", a=16)
        wload_into(wg2[:, :, 0:256], w_in[:, GF_OFF + h * 256:GF_OFF + (h + 1) * 256].rearrange("(p j) f -> p j f", j=16), ("ring", slB), slB)
        wload_into(wg2[:, :, 256:512], w_in[:, GB_OFF + h * 256:GB_OFF + (h + 1) * 256].rearrange("(p j) f -> p j f", j=16), ("ring", slB), slB)
        rA = ("ring", slA); rB = ("ring", slB)
        if h == NH - 1:
            pre_m4_sl = ring_next()
            pre_m4_w = wload(pre_m4_sl, 0, wro[:, 0:512].rearrange("(c p) f -> p c f", p=128), (16, 512), ("ring", pre_m4_sl))

        kv_i = [0]

        def tm_kv(srcT, t, rope_tile, wtab, wcols, dst_f, dst_b, dst_v):
            kb_ = kv_i[0] % 2
            kv_i[0] += 1
            for j in range(16):
                mm(PS[kb_][:, 0:384], srcT[:, j, t * 128:(t + 1) * 128], wkv[:, j, :], j == 0, j == 15, ["hT", rA], [("ps", kb_)])
            if rope_tile is None:
                cp("act", krt, PS[kb_][:, 0:128], [("ps", kb_)], ["krt"])
            else:
                cp("act", kb16, PS[kb_][:, 0:128], [("ps", kb_)], ["kb16"])
                tt("dve", krt[:, 0:64], kb16[:, 0:64], cos_tm[:, rope_tile, :], ALU.mult, ["kb16", "cb"], ["krt"])
                tt("dve", krt[:, 64:128], kb16[:, 64:128], cos_tm[:, rope_tile, :], ALU.mult, ["kb16", "cb"], ["krt"])
                tt("dve", kr2[:, 0:64], kb16[:, 64:128], ss_tm[:, rope_tile, 0:64], ALU.mult, ["kb16", "cb"], ["kr2"])
                tt("dve", kr2[:, 64:128], kb16[:, 0:64], ss_tm[:, rope_tile, 64:128], ALU.mult, ["kb16", "cb"], ["kr2"])
                tt("dve", krt, krt, kr2, ALU.add, ["krt", "kr2"], ["krt"])
            nms = wcols if wcols is not None else (("kw", 0), ("kw", 1), "vb")
            ts("dve", dst_f, krt, wtab[0], None, ALU.mult, None, ["krt", "Wtab"], [nms[0]])
            ts("dve", dst_b, krt, wtab[1], None, ALU.mult, None, ["krt", "Wtab"], [nms[1]])
            cp("act", dst_v, PS[kb_][:, 128:384], [("ps", kb_)], [nms[2]])

        if lim >= 1:
            seq = [("c", t) for t in range(NC)] + [("o", t) for t in range(NT)]
            for si, (kind, t) in enumerate(seq):
                if kind == "c":
                    tm_kv(hcT, t, None, (Wc[:, t, h:h + 1], Wc[:, t, 8 + h:9 + h]), None, kwt[0], kwt[1], vbt)
                else:
                    tm_kv(hTo, t, NT + t, (Wo[:, t, h:h + 1], Wo[:, t, 8 + h:9 + h]), None, kwt[0], kwt[1], vbt)
                for d_ in range(2):
                    if _os.environ.get("SKIP_STATE"):
                        continue
                    mm(PS[2 + d_][:, 0:256], kwt[d_], vbt, True, True, [("kw", d_), "vb"], [("ps", 2 + d_)])
                    if si == 0:
                        cp("dve", R32[d_], PS[2 + d_][:, 0:256], [("ps", 2 + d_)], [("R32", d_)])
                    else:
                        tt("dve", R32[d_], R32[d_], PS[2 + d_][:, 0:256], ALU.add, [("R32", d_), ("ps", 2 + d_)], [("R32", d_)])
            for d_ in range(2):
                cp("act", R16[d_], R32[d_], [("R32", d_)], [("R16", d_)])

        if lim >= 2:
            for (wsrc, raw, rot, nm) in ((wq, qraw, qrot, "q"), (wk, kraw, krot, "k")):
                for bi, (t0, tsz) in enumerate(cfg.TB):
                    for j in range(16):
                        mm(PS[4][:, 0:tsz], wsrc[:, j, :], hTw[:, j, t0:t0 + tsz], j == 0, j == 15, [rA, "hT"], [("ps", 4)])
                    cp("act", raw[:, 0:tsz], PS[4][:, 0:tsz], [("ps", 4)], ["raw" + nm])
                    mm(PS[5][:, 0:tsz], pswap, raw[:, 0:tsz], True, True, ["cb", "raw" + nm], [("ps", 5)])
                    tt("dve", rt1[:, 0:tsz], raw[:, 0:tsz], cosT[:, t0:t0 + tsz], ALU.mult, ["raw" + nm, "cb"], ["rt1"])
                    cp("act", sw16[:, 0:tsz], PS[5][:, 0:tsz], [("ps", 5)], ["sw16"])
                    tt("dve", rt2[:, 0:tsz], sw16[:, 0:tsz], sinS[:, t0:t0 + tsz], ALU.mult, ["sw16", "cb"], ["rt2"])
                    tt("dve", rot[:, t0:t0 + tsz], rt1[:, 0:tsz], rt2[:, 0:tsz], ALU.add, ["rt1", "rt2"], ["rot" + nm])
            nch = TOK // 128
            for (dst, q_) in ((qxf, h), (qxb, 8 + h)):
                for c_ in range(nch):
                    tt("dve", dst[:, c_ * 128:(c_ + 1) * 128], qrot[:, c_ * 128:(c_ + 1) * 128], Xi[:, q_, :], ALU.mult,
                       ["rotq", "Xi"], ["qx%d" % (q_ // 8)])

        if lim >= 3:
            for t in range(NT):
                tm_kv(hTw, t, t, (Zt[:, h:h + 1], Zt[:, 8 + h:9 + h]), (("kzf", t), ("kzb", t), ("vown", t)),
                      kzf[:, t, :], kzb[:, t, :], vown[:, t, :])

        if lim >= 4:
            for t in range(NT):
                pb = 4 + (t % 2)
                for j in range(16):
                    mm(PS[pb][:, 0:512], hTw[:, j, t * 128:(t + 1) * 128], wg2[:, j, :], j == 0, j == 15, ["hT", rB], [("ps", pb)])
                act(sgf[:, t, :], PS[pb][:, 0:256], AF.Silu, [("ps", pb)], [("sgf", t)])
                act(sgb[:, t, :], PS[pb][:, 256:512], AF.Silu, [("ps", pb)], [("sgb", t)])

        if lim >= 5:
            for c in range(NT):
                pb = 4 + (c % 2)
                mm(PS[pb][:, 0:128], krot[:, c * 128:(c + 1) * 128], qrot[:, c * 128:(c + 1) * 128], True, True, ["rotk", "rotq"], [("ps", pb)])
                cp("act", st16, PS[pb][:, 0:128], [("ps", pb)], ["st16"])
                tt("dve", SfT[:, c, :], st16, Dm[:, h, :], ALU.mult, ["st16", "Dm"], [("SfT", c)])
                tt("dve", SbT[:, c, :], st16, Dm[:, 8 + h, :], ALU.mult, ["st16", "Dm"], [("SbT", c)])

        if lim >= 6:
            for step in range(NT):
                for d_ in range(2):
                    c = step if d_ == 0 else NT - 1 - step
                    ST = SfT if d_ == 0 else SbT
                    qx = qxf if d_ == 0 else qxb
                    kz = kzf if d_ == 0 else kzb
                    yb = ybf if d_ == 0 else ybb
                    po = d_
                    mm(PS[po][:, 0:256], ST[:, c, :], vown[:, c, :], True, True, [("SfT" if d_ == 0 else "SbT", c), ("vown", c)], [("ps", po)])
                    mm(PS[4 + d_][:, 0:256], qx[:, c * 128:(c + 1) * 128], R16[d_], True, True, ["qx%d" % d_, ("R16", d_)], [("ps", 4 + d_)])
                    ytmp = rt2[:, d_ * 256:(d_ + 1) * 256]
                    cp("dve", ytmp, PS[po][:, 0:256], [("ps", po), "rt2"], ["rt2", ("yt", d_)])
                    tt("dve", ytmp, ytmp, PS[4 + d_][:, 0:256], ALU.add, [("yt", d_), ("ps", 4 + d_)], ["rt2", ("yt", d_)])
                    S.op("dve", lambda e, d_=d_, c=c, ytmp=ytmp: e.bn_stats(out=st6[:, d_ * NT + c, :], in_=ytmp),
                         reads=[("yt", d_)], writes=[("st6", d_, c)])
                    S.op("dve", lambda e, d_=d_, c=c: e.bn_aggr(out=mv[:, d_ * NT + c, :], in_=st6[:, d_ * NT + c, :]),
                         reads=[("st6", d_, c)], writes=[("mv", d_, c)])
                    cp("act", yb[:, c, :], ytmp, [("yt", d_), "rt2"], [("yb", d_, c)])
                    if step < NT - 1:
                        mm(PS[2 + d_][:, 0:256], kz[:, c, :], vown[:, c, :], True, True, [("kzf" if d_ == 0 else "kzb", c), ("vown", c)], [("ps", 2 + d_)])
                        S.op("dve", lambda e, d_=d_, h=h: e.scalar_tensor_tensor(out=R32[d_], in0=R32[d_], scalar=G128[:, d_ * 8 + h:d_ * 8 + h + 1],
                                                                            in1=PS[2 + d_][:, 0:256], op0=ALU.mult, op1=ALU.add),
                             reads=[("R32", d_), ("ps", 2 + d_), "G128"], writes=[("R32", d_)])
                        cp("act", R16[d_], R32[d_], [("R32", d_)], [("R16", d_)])
        if lim >= 7:
            allmv = [("mv", d_, c) for d_ in range(2) for c in range(NT)]
            act(rsd, mv[:, :, 1], AF.Ln, allmv, ["rsd"], bias=EPS)
            act(rsd, rsd, AF.Exp, ["rsd"], ["rsd"], scale=-0.5)
            S.op("dve", lambda e: e.scalar_tensor_tensor(out=nmr, in0=mv[:, :, 0], scalar=-1.0, in1=rsd, op0=ALU.mult, op1=ALU.mult),
                 reads=allmv + ["rsd"], writes=["nmr"])
            for c in range(NT):
                for d_ in range(2):
                    yb = ybf if d_ == 0 else ybb
                    sg = sgf if d_ == 0 else sgb
                    i_ = d_ * NT + c
                    ts("dve", zt[d_], yb[:, c, :], rsd[:, i_:i_ + 1], nmr[:, i_:i_ + 1], ALU.mult, ALU.add,
                       [("yb", d_, c), "rsd", "nmr"], [("zt", d_)])
                    tt("dve", zt[d_], zt[d_], sg[:, c, :], ALU.mult, [("zt", d_), ("sgf" if d_ == 0 else "sgb", c)], [("zt", d_)])
                tt("dve", ybf[:, c, :], zt[0], zt[1], ALU.add, [("zt", 0), ("zt", 1)], [("yb", 0, c)])
                for k2 in range(2):
                    S.op("pe", lambda e, c=c, k2=k2: e.transpose(out=PSB[:, (c % 2) * 1024 + k2 * 128:(c % 2) * 1024 + (k2 + 1) * 128],
                                                                 in_=ybf[:, c, k2 * 128:(k2 + 1) * 128], identity=ident),
                         reads=[("yb", 0, c), "cb"], writes=[("psb", c % 2)])
                for k2 in range(2):
                    cp("act", retT[:, k2, c * 128:(c + 1) * 128], PSB[:, (c % 2) * 1024 + k2 * 128:(c % 2) * 1024 + (k2 + 1) * 128],
                       [("psb", c % 2)], RT_AL)
            for k2 in range(2):
                dma("sp", rscr[2 * h + k2, :, :], retT[:, k2, :], RT_AL, ["rscr"], "retT")
        if lim < 99:
            break
    S.barrier()

    if debug and debug[0] == "stop_m3":
        tmpd = Reg(R_C.base, R_C.size).get(TOK, F32)
        cp("dve", tmpd, retT[:, 0, :], [], ["tmpd"])
        S.barrier()
        dma("sp", dbg_d, tmpd, [], ["dbg"], "dbg")
        S.barrier(); S.emit(); es.close()
        return nc

    acc = arena[:, R_H.base:R_H.base + 2 * NT * D].bitcast(F32).rearrange("p (t f) -> p t f", t=NT)
    R_S.reset()
    rTb = arena[:, R_H.base:R_H.base + 16 * TOK].rearrange("p (j t) -> p j t", j=16)
    mTb = R_S.get(16 * TOK, BF16, (16, TOK))
    mpart = [R_S.get(512, BF16) for _ in range(2)]
    sgt = [R_S.get(512, F32) for _ in range(2)]
    s16 = [R_S.get(512, BF16) for _ in range(2)]
    xres = [R_S.get(512, F32) for _ in range(2)]
    for j in range(16):
        dma("sp", rTb[:, j, :], rscr[j, :, :], [], ["rTb"], "rTb")
    mi = 0
    for ob in range(4):
        if ob == 0:
            slr, wrb = pre_m4_sl, pre_m4_w
        else:
            slr = ring_next()
            wrb = wload(slr, 0, wro[:, ob * 512:(ob + 1) * 512].rearrange("(c p) f -> p c f", p=128), (16, 512), ("ring", slr))
        slm = ring_next()
        wmr = wload(slm, 0, w_in[:, MR_OFF + ob * 512:MR_OFF + (ob + 1) * 512].rearrange("(p j) f -> p j f", j=16), (16, 512), ("ring", slm))
        if ob == 3:
            pre_m5_sl = ring_next()
            pre_m5_w = wload(pre_m5_sl, 0, wout[:, 0:512].rearrange("(c p) f -> p c f", p=128), (16, 512), ("ring", pre_m5_sl))
        for o4 in range(4):
            oc = ob * 4 + o4
            for bi, (t0, tsz) in enumerate(cfg.TB):
                k2 = mi % 2
                pa, pb = k2 * 2, k2 * 2 + 1
                dma("sp", mpart[k2][:, 0:tsz], mscr[oc, :, t0:t0 + tsz], [], [("mpart", k2)], "mp%d" % k2)
                for c in range(16):
                    mm(PS[pa][:, 0:tsz], wrb[:, c, o4 * 128:(o4 + 1) * 128], rTb[:, c, t0:t0 + tsz], c == 0, c == 15, [("ring", slr), "rTb"], [("ps", pa)])
                for j in range(16):
                    mm(PS[pb][:, 0:tsz], wmr[:, j, o4 * 128:(o4 + 1) * 128], hTw[:, j, t0:t0 + tsz], j == 0, j == 15, [("ring", slm), "hT"], [("ps", pb)])
                act(sgt[k2][:, 0:tsz], PS[pb][:, 0:tsz], AF.Sigmoid, [("ps", pb)], [("sgt", k2)])
                tt("dve", s16[k2][:, 0:tsz], PS[pa][:, 0:tsz], sgt[k2][:, 0:tsz], ALU.mult, [("ps", pa), ("sgt", k2)], [("s16", k2)])
                tt("dve", mTb[:, oc, t0:t0 + tsz], s16[k2][:, 0:tsz], mpart[k2][:, 0:tsz], ALU.add, [("s16", k2), ("mpart", k2)], [("mTb", oc)])
                mi += 1
    S.barrier()
    for ob in range(4):
        if ob == 0:
            slo, wob = pre_m5_sl, pre_m5_w
        else:
            slo = ring_next()
            wob = wload(slo, 0, wout[:, ob * 512:(ob + 1) * 512].rearrange("(c p) f -> p c f", p=128), (16, 512), ("ring", slo))
        for t in range(NT):
            k2 = (ob * NT + t) % 2
            pa = 4 + k2
            dma("sp", xres[k2], x_own[t * 128:(t + 1) * 128, ob * 512:(ob + 1) * 512], [], [("xres", k2)], "xr%d" % k2)
            for c in range(16):
                mm(PS[pa][:, :], mTb[:, c, t * 128:(t + 1) * 128], wob[:, c, :], c == 0, c == 15, [("mTb", c), ("ring", slo)], [("ps", pa)])
            tt("dve", acc[:, t, ob * 512:(ob + 1) * 512], PS[pa][:, :], g1b[:, ob * 512:(ob + 1) * 512], ALU.mult,
               [("ps", pa), "g1b"], [("acc", t, ob)])
            tt("dve", acc[:, t, ob * 512:(ob + 1) * 512], acc[:, t, ob * 512:(ob + 1) * 512], xres[k2], ALU.add,
               [("acc", t, ob), ("xres", k2)], [("acc", t, ob)])
    S.barrier()

    if debug and debug[0] == "x1":
        dma("sp", dbg_d.rearrange("(t p) f -> p t f", p=128), acc, [], ["dbg"], "dbg")
        S.barrier(); S.emit(); es.close()
        return nc

    R_S.reset()
    hx2T = R_S.get(16 * TOK, BF16, (16, TOK))
    hidT = [R_S.get(4 * TOK, BF16, (4, TOK)) for _ in range(2)]
    RC = Reg(R_C.base, R_C.size)
    sgm = [RC.get(512, F32) for _ in range(2)]
    cw = RC.get(NT * NE, F32, (NT, NE))
    rw = RC.get(16 * NR, BF16, (16, NR))
    brt = RC.get(NR, F32)
    lgt_all = RC.get(NT * NR, F32, (NT, NR))
    r8 = RC.get(8, F32); m8 = RC.get(8, F32); rt_a = RC.get(8, F32); rt_b = RC.get(8, F32)
    oh = RC.get(NG, F32); gs = RC.get(4, F32)
    dma("pool", rw, w_rt.rearrange("(p j) f -> p j f", j=16), [], ["rw"], "rw")
    dma("sp", brt, b_rt.partition_broadcast(128), [], ["brt"], "c3")
    dma("sp", g1b, fng.partition_broadcast(128), ["g1b"], ["fngb"], "c3")
    xsb2 = [arena[:, R_W.base + i * D:R_W.base + (i + 1) * D] for i in range(2)]
    junk2 = arena[:, R_W.base + 2 * D:R_W.base + 3 * D]
    ssq2 = RC.get(NT, F32)
    pre_wg = wload(1, 0, wg_d[0].rearrange("(p j) f -> p j f", j=16), (16, FF), ("ring", 1))
    pre_wu = wload(2, 0, wu_d[0].rearrange("(p j) f -> p j f", j=16), (16, FF), ("ring", 2))
    def norm_part(t):
        b = t % 2
        accT = acc[:, t, :]
        racc = [("acc", t, ob) for ob in range(4)]
        sq = ssq2[:, t:t + 1]
        act(junk2, accT, AF.Square, racc, ["junk2", ("ssq2", t)], accum_out=sq)
        act(sq, sq, AF.Ln, [("ssq2", t)], [("ssq2", t)], scale=1.0 / D, bias=EPS)
        act(sq, sq, AF.Exp, [("ssq2", t)], [("ssq2", t)], scale=-0.5)
        act(xsb2[b], accT, AF.Copy, racc + [("ssq2", t)], [("xsb2", b)], scale=sq)
        for j in range(16):
            S.op("pe", lambda e, j=j, b=b: e.transpose(out=PSB[:, j * 128:(j + 1) * 128], in_=xsb2[b][:, j::16], identity=ident),
                 reads=[("xsb2", b), "cb"], writes=[("psb", j // 8)])
        for j in range(16):
            ts("dve", hx2T[:, j, t * 128:(t + 1) * 128], PSB[:, j * 128:(j + 1) * 128], A2[:, j:j + 1], B2[:, j:j + 1],
               ALU.mult, ALU.add, [("psb", j // 8), "AB"], [("hx2T", t)])
        for j in range(16):
            mm(PS[4][:, 0:NR], hx2T[:, j, t * 128:(t + 1) * 128], rw[:, j, :], j == 0, j == 15, [("hx2T", t), "rw"], [("ps", 4)])
        tt("dve", lgt_all[:, t, :], PS[4][:, 0:NR], brt, ALU.add, [("ps", 4), "brt"], [("lgt", t)])

    def route_part(t):
        lgt = lgt_all[:, t, :]
        S.op("dve", lambda e: e.memset(r8, -1e30), writes=["r8"])
        cp("dve", r8[:, 0:NG], lgt[:, 0:NG], [("lgt", t), "r8"], ["r8"])
        S.op("dve", lambda e: e.max(out=m8, in_=r8), reads=["r8"], writes=["m8"])
        ts("dve", oh, lgt[:, 0:NG], m8[:, 0:1], None, ALU.is_ge, None, [("lgt", t), "m8"], ["oh"])
        ts("dve", rt_a[:, 0:NG], lgt[:, 0:NG], m8[:, 0:1], None, ALU.subtract, None, [("lgt", t), "m8"], ["rt_a"])
        act(rt_a[:, 0:NG], rt_a[:, 0:NG], AF.Exp, ["rt_a"], ["rt_a"])
        S.op("dve", lambda e: e.reduce_sum(out=gs[:, 0:1], in_=rt_a[:, 0:NG], axis=AX.X), reads=["rt_a"], writes=["gs"])
        S.op("dve", lambda e: e.reciprocal(out=gs[:, 1:2], in_=gs[:, 0:1]), reads=["gs"], writes=["gs"])
        ts("dve", rt_b, lgt[:, NG:NG + 8], oh[:, 0:1], None, ALU.mult, None, [("lgt", t), "oh"], ["rt_b"])
        for g in range(1, NG):
            S.op("dve", lambda e, g=g: e.scalar_tensor_tensor(out=rt_b, in0=lgt[:, NG + 8 * g:NG + 8 * g + 8], scalar=oh[:, g:g + 1],
                                                              in1=rt_b, op0=ALU.mult, op1=ALU.add), reads=[("lgt", t), "oh", "rt_b"], writes=["rt_b"])
        S.op("dve", lambda e: e.max(out=m8, in_=rt_b), reads=["rt_b", "m8"], writes=["m8"])
        ts("dve", rt_a, rt_b, m8[:, 0:1], None, ALU.subtract, None, ["rt_b", "m8"], ["rt_a"])
        act(rt_a, rt_a, AF.Exp, ["rt_a"], ["rt_a"])
        ts("dve", r8, rt_b, m8[:, 1:2], None, ALU.is_ge, None, ["rt_b", "m8", "r8"], ["r8"])
        tt("dve", rt_a, rt_a, r8, ALU.mult, ["rt_a", "r8"], ["rt_a"])
        ts("dve", gs[:, 2:3], m8[:, 1:2], m8[:, 0:1], None, ALU.subtract, None, ["m8", "gs"], ["gs"])
        act(gs[:, 2:3], gs[:, 2:3], AF.Exp, ["gs"], ["gs"])
        ts("dve", gs[:, 2:3], gs[:, 2:3], 1.0, None, ALU.add, None, ["gs"], ["gs"])
        S.op("dve", lambda e: e.reciprocal(out=gs[:, 2:3], in_=gs[:, 2:3]), reads=["gs"], writes=["gs"])
        tt("dve", gs[:, 2:3], gs[:, 2:3], gs[:, 1:2], ALU.mult, ["gs"], ["gs"])
        ts("dve", rt_a, rt_a, gs[:, 2:3], None, ALU.mult, None, ["rt_a", "gs"], ["rt_a"])
        for g in range(NG):
            ts("dve", cw[:, t, 8 * g:8 * g + 8], rt_a, oh[:, g:g + 1], None, ALU.mult, None, ["rt_a", "oh"], [("cw", t)])

    for t in range(NT):
        norm_part(t)
        if t > 0:
            route_part(t - 1)
    route_part(NT - 1)
    S.barrier()
    ringb.append(arena[:, R_K.base:R_K.base + 16 * K])
    NSLOT = 4 if DEAD_END >= 16 * K else 3
    assert NSLOT == 4
    ring_i[0] = 1

    def ring_next4():
        i = ring_i[0] % NSLOT
        ring_i[0] += 1
        return i
    for ex in range(NE):
        s1, s2, s3 = ring_next4(), ring_next4(), ring_next4()
        if ex == 0:
            assert (s1, s2, s3) == (1, 2, 3)
            wg, wu = pre_wg, pre_wu
        else:
            wg = wload(s1, 0, wg_d[ex].rearrange("(p j) f -> p j f", j=16), (16, FF), ("ring", s1))
            wu = wload(s2, 0, wu_d[ex].rearrange("(p j) f -> p j f", j=16), (16, FF), ("ring", s2))
        wd = wload(s3, 0, wd_d[ex].rearrange("(c p) f -> p c f", p=128), (4, D), ("ring", s3))
        for fc_ in range(4):
            tt("dve", wd[:, fc_, :], wd[:, fc_, :], g2b, ALU.mult, [("ring", s3), "g2b"], [("ring", s3)])
        hb = hidT[ex % 2]
        hn = ("hid", ex % 2)
        mi = 0
        for bi, (t0, tsz) in enumerate(cfg.TB):
            for fc in range(4):
                k2 = mi % 2
                pa, pb = k2 * 2, k2 * 2 + 1
                for j in range(16):
                    mm(PS[pa][:, 0:tsz], wg[:, j, fc * 128:(fc + 1) * 128], hx2T[:, j, t0:t0 + tsz], j == 0, j == 15, [("ring", s1), "hx2"], [("ps", pa)])
                for j in range(16):
                    mm(PS[pb][:, 0:tsz], wu[:, j, fc * 128:(fc + 1) * 128], hx2T[:, j, t0:t0 + tsz], j == 0, j == 15, [("ring", s2), "hx2"], [("ps", pb)])
                act(sgm[k2][:, 0:tsz], PS[pa][:, 0:tsz], AF.Silu, [("ps", pa)], [("sgm", k2)])
                tt("dve", hb[:, fc, t0:t0 + tsz], PS[pb][:, 0:tsz], sgm[k2][:, 0:tsz], ALU.mult, [("ps", pb), ("sgm", k2)], [hn])
                mi += 1
        for t in range(NT):
            for ob in range(4):
                pa = 4 + (t * 4 + ob) % 2
                for fc in range(4):
                    mm(PS[pa][:, :], hb[:, fc, t * 128:(t + 1) * 128], wd[:, fc, ob * 512:(ob + 1) * 512], fc == 0, fc == 3, [hn, ("ring", s3)], [("ps", pa)])
                S.op("dve", lambda e, t=t, ob=ob, pa=pa, ex=ex: e.scalar_tensor_tensor(
                    out=acc[:, t, ob * 512:(ob + 1) * 512], in0=PS[pa][:, :], scalar=cw[:, t, ex:ex + 1],
                    in1=acc[:, t, ob * 512:(ob + 1) * 512], op0=ALU.mult, op1=ALU.add),
                    reads=[("ps", pa), ("cw", t), ("acc", t, ob)], writes=[("acc", t, ob)])
    S.barrier()
    for t in range(NT):
        racc = [("acc", t, ob) for ob in range(4)]
        sq = ssq2[:, t:t + 1]
        b = t % 2
        act(junk2, acc[:, t, :], AF.Square, racc + ["junk2"], ["junk2", ("ssq3", t)], accum_out=sq)
        act(sq, sq, AF.Ln, [("ssq3", t)], [("ssq3", t)], scale=1.0 / D, bias=EPS)
        act(sq, sq, AF.Exp, [("ssq3", t)], [("ssq3", t)], scale=-0.5)
        S.op("dve", lambda e, t=t, sq=sq: e.scalar_tensor_tensor(out=acc[:, t, :], in0=acc[:, t, :], scalar=sq, in1=g1b,
                                                                 op0=ALU.mult, op1=ALU.mult),
             reads=racc + [("ssq3", t), "fngb"], writes=racc)
        dma("sp", out_d[t * 128:(t + 1) * 128, :], acc[:, t, :], racc, ["out"], "outst")
    S.barrier()
    S.emit()
    es.close()
    return nc


def make_in_maps(cfg, x, c, ctx, c_ctx, w_mod, b_mod, norm1_g, norm2_g, w_in, w_four_out, w_ret_out, w_out,
                 ret_decay_f, ret_decay_b, w_group_router, b_group_router, w_expert_router, b_expert_router,
                 w_gate, w_up, w_down, final_norm_g):
    f32 = lambda a: np.ascontiguousarray(np.asarray(a, dtype=np.float32))
    x = f32(x); ctx = f32(ctx); c = f32(c)
    NG = cfg.NG
    w_rt = np.concatenate([f32(w_group_router[0]), f32(w_expert_router[0]).transpose(1, 0, 2).reshape(D, NG * 8)], axis=1)
    b_rt = np.concatenate([f32(b_group_router[0]), f32(b_expert_router[0]).reshape(-1)])
    decay = np.concatenate([f32(ret_decay_f[0]), f32(ret_decay_b[0])])
    shared = {
        "c_ctx": f32(c_ctx), "w_mod": f32(w_mod[0]), "b_mod": f32(b_mod[0]), "norm1_g": f32(norm1_g[0]), "norm2_g": f32(norm2_g[0]),
        "final_norm_g": f32(final_norm_g), "w_in": f32(w_in[0]), "w_four_out": f32(w_four_out[0]), "w_ret_out": f32(w_ret_out[0]),
        "w_out": f32(w_out[0]), "decay": decay, "w_router": np.ascontiguousarray(w_rt), "b_router": np.ascontiguousarray(b_rt),
        "w_gate": f32(w_gate[0]), "w_up": f32(w_up[0]), "w_down": f32(w_down[0]),
    }
    tabs = [host_tables(cfg, half) for half in range(2)]
    maps = []
    TOK = cfg.TOK
    for b in range(cfg.B):
        for half in range(2):
            dft, cb, cf = tabs[half]
            m = dict(shared)
            m["x_own"] = np.ascontiguousarray(x[b, half * TOK:(half + 1) * TOK])
            m["x_oth"] = np.ascontiguousarray(x[b, (1 - half) * TOK:(2 - half) * TOK])
            m["ctx"] = np.ascontiguousarray(ctx[b])
            m["cvec"] = np.ascontiguousarray(c[b])
            m["dft"] = dft; m["cbf"] = cb; m["cf32"] = cf
            maps.append(m)
    return maps


_CACHE = {}


def kernel(**inputs):
    cfg = Cfg()
    if "nc" not in _CACHE:
        _CACHE["nc"] = build(cfg)
    maps = make_in_maps(cfg, **inputs)
    res = run_bass_kernel_spmd(_CACHE["nc"], maps, core_ids=list(range(8)))
    out = np.empty((cfg.B, cfg.SEQ, D), np.float32)
    for b in range(cfg.B):
        for half in range(2):
            out[b, half * cfg.TOK:(half + 1) * cfg.TOK] = res.results[b * 2 + half]["out"]
    return out
```

```python
import math
from contextlib import ExitStack
import numpy as np
import ml_dtypes
import concourse.bass as bass
import concourse.mybir as mybir
from concourse.bass_utils import run_bass_kernel_spmd

F32 = mybir.dt.float32
BF16 = mybir.dt.bfloat16
ALU = mybir.AluOpType
AF = mybir.ActivationFunctionType
AX = mybir.AxisListType

ENGS = ("pe", "act", "dve", "pool", "sp")
SAME_ENGINE_SYNC = {"act", "dve", "pool"}


class Sched:
    def __init__(self, nc):
        self.nc = nc
        self.prog = {e: [] for e in ENGS}
        self.cnt = {e: 0 for e in ENGS}
        self.clock = {e: {} for e in ENGS}
        self.res = {}
        self.chan_cnt = {}
        self.semkeys = list(ENGS)

    def _need(self, eng, tok, waits, same_ok):
        if tok is None:
            return
        key, val, src = tok
        if src == "dma":
            val = self.chan_cnt[key]
        if src == eng and key == eng and eng not in SAME_ENGINE_SYNC:
            return
        if self.clock[eng].get(key, 0) >= val:
            return
        waits[key] = max(waits.get(key, 0), val)

    def op(self, eng, fn, reads=(), writes=(), chan=None, inc=True):
        waits = {}
        for r in reads:
            st = self.res.get(r)
            if st:
                self._need(eng, st[0], waits, False)
        for w in writes:
            st = self.res.get(w)
            if st:
                self._need(eng, st[0], waits, False)
                for t in st[1]:
                    self._need(eng, t, waits, True)
        for k, v in waits.items():
            self.clock[eng][k] = v
        if chan is None and not inc:
            assert eng == "pe"
            tok = (eng, self.cnt[eng] + 1, eng)
            inc = None
        elif chan is None:
            self.cnt[eng] += 1
            tok = (eng, self.cnt[eng], eng)
            inc = (eng, 1)
        else:
            if chan not in self.chan_cnt:
                self.chan_cnt[chan] = 0
                self.semkeys.append(chan)
            self.chan_cnt[chan] += 16
            tok = (chan, self.chan_cnt[chan], "dma")
            inc = (chan, 16)
        self.prog[eng].append((sorted(waits.items(), key=str), fn, inc))
        for r in reads:
            self.res.setdefault(r, [None, []])[1].append(tok)
        for w in writes:
            self.res[w] = [tok, []]
        return tok

    def barrier(self):
        tot = {e: self.cnt[e] for e in ENGS}
        tot.update(self.chan_cnt)
        for e in ENGS:
            waits = {}
            for k, v in tot.items():
                if v > 0 and self.clock[e].get(k, 0) < v and not (k == e and e in ("pe", "sp")):
                    waits[k] = v
                    self.clock[e][k] = v
            if waits:
                self.prog[e].append((sorted(waits.items(), key=str), None, None))
        self.res = {}

    def emit(self):
        nc = self.nc
        with ExitStack() as es:
            sems = {k: es.enter_context(nc.semaphore("s_" + str(k))) for k in self.semkeys}
            block = es.enter_context(nc.Block())

            def run(name):
                def body(e):
                    for waits, fn, inc in self.prog[name]:
                        for k, v in waits:
                            e.wait_ge(sems[k], v)
                        if fn is not None:
                            ins = fn(e)
                            if inc is not None:
                                ins.then_inc(sems[inc[0]], inc[1])
                return body

            block.tensor(run("pe"))
            block.scalar(run("act"))
            block.vector(run("dve"))
            block.gpsimd(run("pool"))
            block.sync(run("sp"))


D = 2048
NJ = 16
FW = 1024
NH = 8
DK = 128
DV = 256
Q_OFF = FW
K_OFF = Q_OFF + NH * DK
V_OFF = K_OFF + NH * DK
GF_OFF = V_OFF + NH * DV
GB_OFF = GF_OFF + NH * DV
MF_OFF = GB_OFF + NH * DV
MR_OFF = MF_OFF + D
IN_W = MR_OFF + D
FF = 512
EPS = 1e-6
GRID_W = 64
ROPE_BASE = 10000.0


class Cfg:
    def __init__(self, seq=2048, ctx=256, ngroups=4, batch=4):
        self.SEQ, self.CTX, self.NG, self.B = seq, ctx, ngroups, batch
        self.TOK = seq // 2
        self.NT = self.TOK // 128
        self.NC = ctx // 128
        self.NE = ngroups * 8
        self.NR = ngroups + self.NE
        self.TB = [(t0, min(512, self.TOK - t0)) for t0 in range(0, self.TOK, 512)]


def host_tables(cfg, half):
    TOK, SEQ, NT = cfg.TOK, cfg.SEQ, cfg.NT
    own = np.arange(half * TOK, (half + 1) * TOK)
    oth = np.arange((1 - half) * TOK, (2 - half) * TOK)
    order = np.concatenate([own, oth])
    ang = 2.0 * np.pi * ((order[:, None].astype(np.int64) * own[None, :].astype(np.int64)) % SEQ) / SEQ
    dft = np.stack([np.cos(ang), np.sin(ang)], axis=1).astype(ml_dtypes.bfloat16)
    f = np.arange(128)
    angg = 2.0 * np.pi * ((f[:, None] * f[None, :]) % 128) / 128.0
    n_freq = DK // 4
    inv = ROPE_BASE ** (-np.arange(n_freq, dtype=np.float32) / n_freq)

    def rope(pos):
        row = (pos // GRID_W).astype(np.float32)
        col = (pos % GRID_W).astype(np.float32)
        a = np.concatenate([row[:, None] * inv, col[:, None] * inv], axis=-1).astype(np.float32)
        return np.cos(a), np.sin(a)

    co, so = rope(own)
    NTT = 2 * NT
    CBW = 640 + 2 * TOK + NTT * 64 + NTT * 128
    cb = np.zeros((128, CBW), ml_dtypes.bfloat16)
    cb[:, 0:128] = np.eye(128)
    psw = np.zeros((128, 128), np.float32)
    for m in range(128):
        psw[(m + 64) % 128, m] = 1.0
    cb[:, 128:256] = psw
    cb[:, 256:384] = np.cos(angg)
    cb[:, 384:512] = -np.sin(angg)
    cb[:, 512:640] = 0
    cosT = np.concatenate([co.T, co.T], axis=0)
    sinS = np.concatenate([-so.T, so.T], axis=0)
    cb[:, 640:640 + TOK] = cosT
    cb[:, 640 + TOK:640 + 2 * TOK] = sinS
    call, sall = rope(order)
    cc = call.reshape(NTT, 128, 64).transpose(1, 0, 2)
    ss = np.concatenate([-sall, sall], axis=1).reshape(NTT, 128, 128).transpose(1, 0, 2)
    o_ = 640 + 2 * TOK
    cb[:, o_:o_ + NTT * 64] = cc.reshape(128, -1)
    cb[:, o_ + NTT * 64:o_ + NTT * 64 + NTT * 128] = ss.reshape(128, -1)
    j = np.arange(128)[:, None].astype(np.float32)
    i = np.arange(128)[None, :].astype(np.float32)
    s = DK ** -0.5
    dmat = (i - j) * np.ones((128, 128), np.float32)
    mf = (i >= j).astype(np.float32) * s
    mb = (j >= i).astype(np.float32) * s
    pr1 = (i + 1.0) * np.ones((128, 1), np.float32)
    prb = (128.0 - i) * np.ones((128, 1), np.float32)
    m = np.arange(128, dtype=np.float32)
    MT = max(cfg.NC, NT)
    pc = np.zeros((128, 3, MT), np.float32)
    for t in range(MT):
        pc[:, 0, t] = cfg.CTX - 1 - (128 * t + m)
        pc[:, 1, t] = 128 * t + m
        pc[:, 2, t] = TOK - 1 - (128 * t + m)
    pz = np.stack([127.0 - m, m], axis=1)
    fl = np.zeros((128, 16), np.float32)
    fl[:, 0:8] = 1.0 if half == 1 else 0.0
    fl[:, 8:16] = 1.0 if half == 0 else 0.0
    cf = np.concatenate([dmat, mf, mb, pr1, prb,
                         pc.reshape(128, -1), pz, fl], axis=1).astype(np.float32)
    return dft, cb, np.ascontiguousarray(cf)


def build(cfg, debug=None):
    TOK, NT, NC, NE, NG, NR = cfg.TOK, cfg.NT, cfg.NC, cfg.NE, cfg.NG, cfg.NR
    NTT = 2 * NT
    MT = max(NC, NT)
    nc = bass.Bass("TRN2", target_bir_lowering=False)

    def din(name, shape, dt=F32):
        return nc.dram_tensor(name, list(shape), dt, kind="ExternalInput").ap()

    x_own = din("x_own", [TOK, D]); x_oth = din("x_oth", [TOK, D]); ctx_d = din("ctx", [cfg.CTX, D])
    cvec = din("cvec", [D]); cctx = din("c_ctx", [D])
    w_mod = din("w_mod", [D, 6 * D]); b_mod = din("b_mod", [6 * D])
    n1g = din("norm1_g", [D]); n2g = din("norm2_g", [D]); fng = din("final_norm_g", [D])
    w_in = din("w_in", [D, IN_W]); w4 = din("w_four_out", [FW, D]); wro = din("w_ret_out", [D, D]); wout = din("w_out", [D, D])
    decay = din("decay", [16])
    w_rt = din("w_router", [D, NR]); b_rt = din("b_router", [NR])
    wg_d = din("w_gate", [NE, D, FF]); wu_d = din("w_up", [NE, D, FF]); wd_d = din("w_down", [NE, FF, D])
    dft_d = din("dft", [cfg.SEQ, 2, TOK], BF16)
    CBW = 640 + 2 * TOK + NTT * 64 + NTT * 128
    cb_d = din("cbf", [128, CBW], BF16)
    CFW = 5 * 128 + 3 * MT + 2 + 16
    cf_d = din("cf32", [128, CFW])
    out_d = nc.dram_tensor("out", [TOK, D], F32, kind="ExternalOutput").ap()
    mscr = nc.dram_tensor("mscr", [NJ, 128, TOK], BF16, kind="Internal").ap()
    rscr = nc.dram_tensor("rscr", [NJ, 128, TOK], BF16, kind=("ExternalOutput" if (debug and debug[0] == "stop_m3") else "Internal")).ap()
    dbg_d = None
    if debug:
        dbg_d = nc.dram_tensor("dbg", list(debug[1]), F32, kind="ExternalOutput").ap()

    es = ExitStack()
    ARENA = 207 * 512
    arena = es.enter_context(nc.sbuf_tensor("arena", [128, ARENA], BF16))
    PS = [es.enter_context(nc.psum_tensor("ps%d" % i, [128, 512], F32)) for i in range(6)]
    PSB = es.enter_context(nc.psum_tensor("psb", [128, 2048], BF16))
    PSBF = [PSB[:, 0:1024].bitcast(F32), PSB[:, 1024:2048].bitcast(F32)]
    S = Sched(nc)

    class Reg:
        def __init__(self, base, size):
            self.base, self.size, self.off = base, size, 0

        def reset(self):
            self.off = 0

        def get(self, nelem, dt, shape=None):
            n2 = nelem * (2 if dt == F32 else 1)
            if n2 % 2:
                n2 += 1
            assert self.off + n2 <= self.size, ("region overflow", self.off, n2, self.size)
            a = self.base + self.off
            self.off += n2
            ap = arena[:, a:a + n2]
            if dt == F32:
                ap = ap.bitcast(F32)
            if shape is not None:
                if len(shape) == 2:
                    ap = ap.rearrange("p (a b) -> p a b", a=shape[0])
                else:
                    ap = ap.rearrange("p (a b c) -> p a b c", a=shape[0], b=shape[1])
            return ap

    K = 512
    pos = [0]

    def region(kb):
        r = Reg(pos[0], int(kb * K))
        pos[0] += int(kb * K)
        assert pos[0] <= ARENA, pos[0]
        return r

    R_K = region(36)
    R_H = region(64)
    R_C = region(8)
    R_W = region(48)
    R_S = region(51)

    cb = R_K.get(CBW, BF16)
    ident = cb[:, 0:128]; pswap = cb[:, 128:256]; cgm = cb[:, 256:384]; nsg = cb[:, 384:512]
    cosT = cb[:, 640:640 + TOK]; sinS = cb[:, 640 + TOK:640 + 2 * TOK]
    cf = R_K.get(CFW, F32)
    o = 640 + 2 * TOK
    cos_tm = cb[:, o:o + NTT * 64].rearrange("p (t f) -> p t f", t=NTT); o += NTT * 64
    ss_tm = cb[:, o:o + NTT * 128].rearrange("p (t f) -> p t f", t=NTT)
    o = 0
    dmat = cf[:, o:o + 128]; o += 128
    mfm = cf[:, o:o + 128]; o += 128
    mbm = cf[:, o:o + 128]; o += 128
    pr1 = cf[:, o:o + 128]; o += 128
    prb = cf[:, o:o + 128]; o += 128
    pcs = cf[:, o:o + 3 * MT].rearrange("p (a t) -> p a t", a=3); o += 3 * MT
    pz = cf[:, o:o + 2]; o += 2
    fl16 = cf[:, o:o + 16]; o += 16
    Dm = R_K.get(16 * 128, BF16, (16, 128))
    Xi = R_K.get(16 * 128, BF16, (16, 128))
    DEAD_END = R_K.off
    lg = R_K.get(16, F32); nlg = R_K.get(16, F32)
    G128 = R_K.get(16, F32)
    Zt = R_K.get(16, F32)
    Wc = R_K.get(NC * 16, F32, (NC, 16))
    Wo = R_K.get(NT * 16, F32, (NT, 16))
    A1 = R_K.get(16, F32); B1 = R_K.get(16, F32); A1c = R_K.get(16, F32); B1c = R_K.get(16, F32)
    A2 = R_K.get(16, F32); B2 = R_K.get(16, F32)
    g1b = R_K.get(D, F32)
    g2b = R_K.get(D, BF16)
    ones1 = R_K.get(2, F32)
    small = R_K.get(64, F32)

    def dma(eng, out, in_, reads, writes, chan):
        return S.op(eng, lambda e: e.dma_start(out=out, in_=in_), reads=reads, writes=writes, chan=chan)

    def mm(out, lhsT, rhs, start, stop, reads, writes, sig=False):
        return S.op("pe", lambda e: e.matmul(out, lhsT=lhsT, rhs=rhs, start=start, stop=stop), reads=reads, writes=writes,
                    inc=bool(stop or sig))

    def act(out, in_, func, reads, writes, **kw):
        return S.op("act", lambda e: e.activation(out=out, in_=in_, func=func, **kw), reads=reads, writes=writes)

    def tt(eng, out, in0, in1, op, reads, writes):
        return S.op(eng, lambda e: e.tensor_tensor(out=out, in0=in0, in1=in1, op=op), reads=reads, writes=writes)

    def ts(eng, out, in0, s1, s2, op0, op1, reads, writes):
        if s2 is None:
            return S.op(eng, lambda e: e.tensor_scalar(out=out, in0=in0, scalar1=s1, scalar2=None, op0=op0), reads=reads, writes=writes)
        return S.op(eng, lambda e: e.tensor_scalar(out=out, in0=in0, scalar1=s1, scalar2=s2, op0=op0, op1=op1), reads=reads, writes=writes)

    def cp(eng, out, in_, reads, writes):
        if eng == "act":
            return S.op("act", lambda e: e.copy(out=out, in_=in_), reads=reads, writes=writes)
        return S.op(eng, lambda e: e.tensor_copy(out=out, in_=in_), reads=reads, writes=writes)

    ring = [R_W.get(16 * K // 1, BF16) for _ in range(3)] if False else None
    ringb = [arena[:, R_W.base + i * 16 * K:R_W.base + (i + 1) * 16 * K] for i in range(3)]
    ring_i = [0]

    def ring_next():
        i = ring_i[0] % 3
        ring_i[0] += 1
        return i

    def wload(slot, off_el, dram_ap, shape3, rname):
        a, b = shape3
        ap = ringb[slot][:, off_el:off_el + a * b].rearrange("p (a b) -> p a b", a=a)
        step = max(1, min(a, 4096 // max(1, 1)))
        step = max(1, a // 4) if a >= 4 else a
        for a0 in range(0, a, step):
            a1 = min(a, a0 + step)
            dma("pool", ap[:, a0:a1, :], dram_ap[:, a0:a1, :], [], [rname], "ring%d" % slot)
        return ap

    def wload_into(dst3, dram_ap, rname, slot):
        a = dst3.shape[1]
        step = max(1, a // 4) if a >= 4 else a
        for a0 in range(0, a, step):
            a1 = min(a, a0 + step)
            dma("pool", dst3[:, a0:a1, :], dram_ap[:, a0:a1, :], [], [rname], "ring%d" % slot)

    dma("sp", cb, cb_d, [], ["cb"], "c0")
    dma("sp", cf, cf_d, [], ["cf"], "c0")
    dec = small[:, 0:16]
    dma("sp", dec, decay.partition_broadcast(128), [], ["dec"], "c0")
    gn1 = small[:, 16:32]; gn2 = small[:, 32:48]
    dma("sp", gn1, n1g.rearrange("(p j) -> p j", j=16), [], ["gn1"], "c0")
    dma("sp", gn2, n2g.rearrange("(p j) -> p j", j=16), [], ["gn2"], "c0")
    S.op("dve", lambda e: e.memset(ones1, 1.0), writes=["ones1"])

    R_S.reset()
    tA = R_S.get(16, F32); tB = R_S.get(16, F32); tC = R_S.get(16, F32); tD = R_S.get(16, F32)
    ts("dve", tB, dec, -1.0, None, ALU.mult, None, ["dec"], ["tB"])
    tt("dve", tA, dec, tB, ALU.max, ["dec", "tB"], ["tA"])
    act(tA, tA, AF.Exp, ["tA"], ["tA"], scale=-1.0)
    ts("dve", tB, tA, 2.0, None, ALU.add, None, ["tA"], ["tB"])
    S.op("dve", lambda e: e.reciprocal(out=tB, in_=tB), reads=["tB"], writes=["tB"])
    tt("dve", tB, tA, tB, ALU.mult, ["tA", "tB"], ["tB"])
    tt("dve", tC, tB, tB, ALU.mult, ["tB"], ["tC"])
    ts("dve", tD, tC, 1.0 / 9.0, 1.0 / 7.0, ALU.mult, ALU.add, ["tC"], ["tD"])
    for cst in (1.0 / 5.0, 1.0 / 3.0, 1.0):
        tt("dve", tD, tD, tC, ALU.mult, ["tD", "tC"], ["tD"])
        ts("dve", tD, tD, cst, None, ALU.add, None, ["tD"], ["tD"])
    tt("dve", tD, tD, tB, ALU.mult, ["tD", "tB"], ["tD"])
    ts("dve", tA, dec, 0.0, None, ALU.min, None, ["dec"], ["tA"])
    S.op("dve", lambda e: e.scalar_tensor_tensor(out=lg, in0=tD, scalar=-2.0, in1=tA, op0=ALU.mult, op1=ALU.add),
         reads=["tD", "tA"], writes=["lg"])
    ts("dve", nlg, lg, -1.0, None, ALU.mult, None, ["lg"], ["nlg"])
    act(G128, lg, AF.Exp, ["lg"], ["G128"], scale=128.0)
    GT = tA
    act(GT, lg, AF.Exp, ["lg"], ["GT"], scale=float(TOK))
    mulc = tB
    ts("dve", mulc, GT, -1.0, None, ALU.add, None, ["GT"], ["mulc"])
    tt("dve", mulc, mulc, fl16, ALU.mult, ["mulc", "cf"], ["mulc"])
    ts("dve", mulc, mulc, 1.0, None, ALU.add, None, ["mulc"], ["mulc"])
    for d_ in range(2):
        cs = slice(d_ * 8, d_ * 8 + 8)
        act(Zt[:, cs], lg[:, cs], AF.Exp, ["lg", "cf"], ["Zt"], scale=pz[:, d_:d_ + 1])
        for t in range(NC):
            act(Wc[:, t, cs], lg[:, cs], AF.Exp, ["lg", "cf"], ["Wc"], scale=pcs[:, (0 if d_ == 0 else 1), t:t + 1])
            tt("dve", Wc[:, t, cs], Wc[:, t, cs], mulc[:, cs], ALU.mult, ["Wc", "mulc"], ["Wc"])
        for t in range(NT):
            act(Wo[:, t, cs], lg[:, cs], AF.Exp, ["lg", "cf"], ["Wo"], scale=pcs[:, (2 if d_ == 0 else 1), t:t + 1])
            tt("dve", Wo[:, t, cs], Wo[:, t, cs], fl16[:, cs], ALU.mult, ["Wo", "cf"], ["Wo"])
    mtmp = R_S.get(128, F32)
    for q in range(16):
        if q < 8:
            act(mtmp, dmat, AF.Exp, ["lg", "cf", "mtmp"], ["mtmp"], scale=lg[:, q:q + 1])
            tt("dve", Dm[:, q, :], mtmp, mfm, ALU.mult, ["mtmp", "cf"], ["Dm"])
            act(mtmp, pr1, AF.Exp, ["lg", "cf", "mtmp"], ["mtmp"], scale=lg[:, q:q + 1])
        else:
            act(mtmp, dmat, AF.Exp, ["nlg", "cf", "mtmp"], ["mtmp"], scale=nlg[:, q:q + 1])
            tt("dve", Dm[:, q, :], mtmp, mbm, ALU.mult, ["mtmp", "cf"], ["Dm"])
            act(mtmp, prb, AF.Exp, ["lg", "cf", "mtmp"], ["mtmp"], scale=lg[:, q:q + 1])
        ts("dve", Xi[:, q, :], mtmp, DK ** -0.5, None, ALU.mult, None, ["mtmp"], ["Xi"])

    vbx = arena[:, R_H.base:R_H.base + 2 * 6 * D].bitcast(F32)
    vbc = arena[:, R_H.base + 2 * 6 * D:R_H.base + 2 * 8 * D].bitcast(F32)
    craw = R_S.get(32, F32)
    dma("sp", craw[:, 0:16], cvec.rearrange("(p j) -> p j", j=16), [], ["craw"], "c1")
    dma("sp", craw[:, 16:32], cctx.rearrange("(p j) -> p j", j=16), [], ["craw"], "c1")
    csil = R_S.get(32, BF16)
    act(csil, craw, AF.Silu, ["craw"], ["csil"])
    screp = R_S.get(32 * 128, BF16, (32, 128))
    cp("dve", screp, csil.unsqueeze(2).to_broadcast([128, 32, 128]), ["csil"], ["screp"])
    bmb = R_S.get(512, F32)
    NB = 6 * D // 512
    for blk in range(NB):
        sl = ring_next()
        wb = wload(sl, 0, w_mod[:, blk * 512:(blk + 1) * 512].rearrange("(p j) f -> p j f", j=16), (16, 512), ("ring", sl))
        dma("sp", bmb, b_mod[blk * 512:(blk + 1) * 512].partition_broadcast(128), [], ["bmb"], "c2")
        for which in range(2 if blk < 8 else 1):
            pb = blk % 2 + 2 * which
            for j in range(16):
                mm(PS[pb][:, :], screp[:, which * 16 + j, :], wb[:, j, :], j == 0, j == 15, ["screp", ("ring", sl)], [("ps", pb)])
            dst = (vbx if which == 0 else vbc)[:, blk * 512:(blk + 1) * 512]
            tt("dve", dst, PS[pb][:, :], bmb, ALU.add, [("ps", pb), "bmb"], ["vb"])
    colv = R_S.get(6 * 16, F32, (6, 16))
    srcs = [(vbx, 0), (vbx, D), (vbc, 0), (vbc, D), (vbx, 3 * D), (vbx, 4 * D)]
    for vi, (vsrc, base) in enumerate(srcs):
        for j in range(16):
            mm(PS[4][:, vi * 16 + j:vi * 16 + j + 1], vsrc[0:1, base + j:base + D:16], ones1[0:1, 0:1], True, True,
               ["vb", "ones1"], [("ps", 4)])
    cp("dve", colv.rearrange("p a b -> p (a b)"), PS[4][:, 0:96], [("ps", 4)], ["colv"])
    for (Adst, Bdst, sidx, shidx, gsrc, gname) in ((A1, B1, 1, 0, gn1, "gn1"), (A1c, B1c, 3, 2, gn1, "gn1"), (A2, B2, 5, 4, gn2, "gn2")):
        S.op("dve", lambda e, Adst=Adst, sidx=sidx, gsrc=gsrc: e.scalar_tensor_tensor(
            out=Adst, in0=colv[:, sidx, :], scalar=1.0, in1=gsrc, op0=ALU.add, op1=ALU.mult),
            reads=["colv", gname], writes=["AB"])
        cp("dve", Bdst, colv[:, shidx, :], ["colv"], ["AB"])
    cp("dve", g1b, vbx[:, 2 * D:3 * D], ["vb"], ["g1b"])
    cp("dve", g2b, vbx[:, 5 * D:6 * D], ["vb"], ["g2b"])
    S.barrier()

    hTo = arena[:, R_H.base:R_H.base + 16 * TOK].rearrange("p (j t) -> p j t", j=16)
    hTw = arena[:, R_H.base + 16 * TOK:R_H.base + 32 * TOK].rearrange("p (j t) -> p j t", j=16)
    hcT = arena[:, R_C.base:R_C.base + 16 * cfg.CTX].rearrange("p (j t) -> p j t", j=16)
    R_S.reset()
    xst = [R_S.get(D, F32) for _ in range(2)]
    xsb = [R_S.get(D, BF16) for _ in range(2)]
    junk = R_S.get(D, BF16)
    ssq = R_S.get(2 * NT + NC, F32)
    pre_m2_sl = ring_next()
    pre_m2_w = wload(pre_m2_sl, 0, w_in[:, 0:256].rearrange("(p j) f -> p j f", j=16), (16, 256), ("ring", pre_m2_sl))
    tiles = [(x_own, t, hTw, A1, B1) for t in range(NT)] + [(x_oth, t, hTo, A1, B1) for t in range(NT)] + \
            [(ctx_d, t, hcT, A1c, B1c) for t in range(NC)]
    for ti, (src, t, dstT, Av, Bv) in enumerate(tiles):
        b = ti % 2
        dma("sp", xst[b], src[t * 128:(t + 1) * 128, :], [], [("xst", b)], "xs%d" % b)
        sq = ssq[:, ti:ti + 1]
        act(junk, xst[b], AF.Square, [("xst", b)], ["junk", ("ssq", ti)], accum_out=sq)
        act(sq, sq, AF.Ln, [("ssq", ti)], [("ssq", ti)], scale=1.0 / D, bias=EPS)
        act(sq, sq, AF.Exp, [("ssq", ti)], [("ssq", ti)], scale=-0.5)
        act(xsb[b], xst[b], AF.Copy, [("xst", b), ("ssq", ti)], [("xsb", b)], scale=sq)
        for j in range(16):
            S.op("pe", lambda e, j=j, b=b: e.transpose(out=PSB[:, j * 128:(j + 1) * 128], in_=xsb[b][:, j::16], identity=ident),
                 reads=[("xsb", b), "cb"], writes=[("psb", j // 8)])
        for j in range(16):
            ts("dve", dstT[:, j, t * 128:(t + 1) * 128], PSB[:, j * 128:(j + 1) * 128], Av[:, j:j + 1], Bv[:, j:j + 1],
               ALU.mult, ALU.add, [("psb", j // 8), "AB"], ["hT"])
    S.barrier()

    def dbg_dump(ap_list):
        for a, d_ in ap_list:
            dma("sp", d_, a, [], ["dbg"], "dbg")
        S.barrier()
        S.emit()
        es.close()
        return nc

    if debug and debug[0] == "hT":
        R_S.reset()
        tmp = R_S.get(16 * 128, F32, (16, 128))
        cp("dve", tmp, hTw[:, :, 0:128], [], ["tmp"])
        S.barrier()
        tmp2 = R_S.get(16 * 128, F32, (16, 128))
        cp("dve", tmp2, hcT[:, :, 0:128], [], ["tmp2"])
        S.barrier()
        return dbg_dump([(tmp, dbg_d[0]), (tmp2, dbg_d[1])])

    inv_scale = 1.0 / math.sqrt(cfg.SEQ * 128.0)
    R_S.reset()
    u_gp = R_S.get(NTT * 256, BF16, (NTT, 256))
    tbl = [R_S.get(2 * TOK, BF16, (2, TOK)) for _ in range(2)]
    PT = R_S.get(4 * TOK, BF16, (4, TOK))
    YT = R_S.get(8 * TOK, BF16, (8, TOK))
    ptmp = [R_S.get(512, F32) for _ in range(2)]
    hsrc = [(hTw, t) for t in range(NT)] + [(hTo, t) for t in range(NT)]
    tbl_i = 0
    for gp in range(4):
        if gp == 0:
            sl, wu = pre_m2_sl, pre_m2_w
        else:
            sl = ring_next()
            wu = wload(sl, 0, w_in[:, gp * 256:(gp + 1) * 256].rearrange("(p j) f -> p j f", j=16), (16, 256), ("ring", sl))
        for ti, (hsrcT, t) in enumerate(hsrc):
            pb = ti % 2
            for j in range(16):
                mm(PS[pb][:, 0:256], hsrcT[:, j, t * 128:(t + 1) * 128], wu[:, j, :], j == 0, j == 15, ["hT", ("ring", sl)], [("ps", pb)])
            cp("act" if ti % 2 else "dve", u_gp[:, ti, :], PS[pb][:, 0:256], [("ps", pb)], [("u_gp", ti)])
        accs = [(g2, cs, bi) for g2 in range(2) for cs in range(2) for bi in range(len(cfg.TB))]
        assert len(accs) <= 8

        def accbank(ai, tsz):
            if ai < 6:
                return PS[ai][:, 0:tsz], ("ps", ai)
            return PSBF[ai - 6][:, 0:tsz], ("psb", ai - 6)
        for ncx in range(NTT):
            tb_ = tbl_i % 2
            tbl_i += 1
            dma("sp", tbl[tb_], dft_d[ncx * 128:(ncx + 1) * 128, :, :], [], [("tbl", tb_)], "tbl%d" % tb_)
            for ai, (g2, cs, bi) in enumerate(accs):
                t0, tsz = cfg.TB[bi]
                outp, rn = accbank(ai, tsz)
                mm(outp, u_gp[:, ncx, g2 * 128:(g2 + 1) * 128], tbl[tb_][:, cs, t0:t0 + tsz], ncx == 0, ncx == NTT - 1,
                   [("u_gp", ncx), ("tbl", tb_)], [rn], sig=True)
        for ai, (g2, cs, bi) in enumerate(accs):
            t0, tsz = cfg.TB[bi]
            outp, rn = accbank(ai, tsz)
            cp("act" if ai % 2 else "dve", PT[:, g2 * 2 + cs, t0:t0 + tsz], outp, [rn], ["PT"])
        for g2 in range(2):
            g = gp * 2 + g2
            for bi, (t0, tsz) in enumerate(cfg.TB):
                pb = 4 + (g2 + bi) % 2
                mm(PS[pb][:, 0:tsz], cgm, PT[:, g2 * 2 + 0, t0:t0 + tsz], True, False, ["cb", "PT"], [("ps", pb)])
                mm(PS[pb][:, 0:tsz], nsg, PT[:, g2 * 2 + 1, t0:t0 + tsz], False, True, ["cb", "PT"], [("ps", pb)])
                act(YT[:, g, t0:t0 + tsz], PS[pb][:, 0:tsz], AF.Copy, [("ps", pb)], ["YT"], scale=inv_scale)
    mst = [R_S.get(512, BF16) for _ in range(2)]
    mi = 0
    for ob in range(4):
        sl4 = ring_next()
        w4b = wload(sl4, 0, w4[:, ob * 512:(ob + 1) * 512].rearrange("(g p) f -> p g f", p=128), (8, 512), ("ring", sl4))
        slm = ring_next()
        wmf = wload(slm, 0, w_in[:, MF_OFF + ob * 512:MF_OFF + (ob + 1) * 512].rearrange("(p j) f -> p j f", j=16), (16, 512), ("ring", slm))
        for o4 in range(4):
            oc = ob * 4 + o4
            for bi, (t0, tsz) in enumerate(cfg.TB):
                pa, pb = (mi % 2) * 2, (mi % 2) * 2 + 1
                for g in range(8):
                    mm(PS[pa][:, 0:tsz], w4b[:, g, o4 * 128:(o4 + 1) * 128], YT[:, g, t0:t0 + tsz], g == 0, g == 7, [("ring", sl4), "YT"], [("ps", pa)])
                for j in range(16):
                    mm(PS[pb][:, 0:tsz], wmf[:, j, o4 * 128:(o4 + 1) * 128], hTw[:, j, t0:t0 + tsz], j == 0, j == 15, [("ring", slm), "hT"], [("ps", pb)])
                k2 = mi % 2
                act(ptmp[k2][:, 0:tsz], PS[pb][:, 0:tsz], AF.Sigmoid, [("ps", pb)], [("ptmp", k2)])
                tt("dve", mst[k2][:, 0:tsz], PS[pa][:, 0:tsz], ptmp[k2][:, 0:tsz], ALU.mult, [("ps", pa), ("ptmp", k2)], [("mst", k2)])
                dma("sp", mscr[oc, :, t0:t0 + tsz], mst[k2][:, 0:tsz], [("mst", k2)], ["mscr"], "mst%d" % k2)
                mi += 1
    S.barrier()

    if debug and debug[0] == "stop_m2":
        R_S.reset()
        tmpd = R_S.get(TOK, F32)
        cp("dve", tmpd, YT[:, 0, :], [], ["tmpd"])
        S.barrier()
        dma("sp", dbg_d, tmpd, [], ["dbg"], "dbg")
        S.barrier(); S.emit(); es.close()
        return nc

    R_S.reset()
    _alias0 = R_S.off
    qraw = R_S.get(512, BF16); kraw = R_S.get(512, BF16)
    rt1 = R_S.get(512, F32); rt2 = R_S.get(512, F32)
    qrot = R_S.get(TOK, BF16); krot = R_S.get(TOK, BF16)
    qxf = R_S.get(TOK, BF16); qxb = R_S.get(TOK, BF16)
    kzf = R_S.get(NT * 128, BF16, (NT, 128)); kzb = R_S.get(NT * 128, BF16, (NT, 128))
    vown = R_S.get(NT * 256, BF16, (NT, 256))
    SfT = R_S.get(NT * 128, BF16, (NT, 128)); SbT = R_S.get(NT * 128, BF16, (NT, 128))
    ybf = R_S.get(NT * 256, BF16, (NT, 256)); ybb = R_S.get(NT * 256, BF16, (NT, 256))
    sgf = R_S.get(NT * 256, BF16, (NT, 256)); sgb = R_S.get(NT * 256, BF16, (NT, 256))
    R32 = [R_S.get(256, F32) for _ in range(2)]
    R16 = [R_S.get(256, BF16) for _ in range(2)]
    krt = R_S.get(128, F32); kr2 = R_S.get(128, F32)
    kwt = [R_S.get(128, BF16) for _ in range(2)]
    vbt = R_S.get(256, BF16)
    st6 = R_S.get(2 * NT * 6, F32, (2 * NT, 6))
    mv = R_S.get(2 * NT * 2, F32, (2 * NT, 2))
    rsd = R_S.get(2 * NT, F32); nmr = R_S.get(2 * NT, F32)
    zt = [R_S.get(256, BF16) for _ in range(2)]
    kb16 = R_S.get(128, BF16); sw16 = R_S.get(512, BF16); st16 = R_S.get(128, BF16)
    assert 2 * TOK <= 3072
    retT = arena[:, R_S.base + _alias0:R_S.base + _alias0 + 2 * TOK].rearrange("p (a b) -> p a b", a=2)
    RT_AL = ["retT", "rawq", "rawk", "rt1", "rt2"]

    lim = debug[2] if (debug and debug[0] == "stop_m3" and len(debug) > 2) else 99
    import os as _os
    for h in range(NH):
        slA = ring_next()
        wq = wload(slA, 0, w_in[:, Q_OFF + h * 128:Q_OFF + (h + 1) * 128].rearrange("(p j) f -> p j f", j=16), (16, 128), ("ring", slA))
        wkv = ringb[slA][:, 16 * 128:16 * 128 + 16 * 384].rearrange("p (a b) -> p a b", a=16)
        wk = wkv[:, :, 0:128]
        wload_into(wk, w_in[:, K_OFF + h * 128:K_OFF + (h + 1) * 128].rearrange("(p j) f -> p j f", j=16), ("ring", slA), slA)
        wload_into(wkv[:, :, 128:384], w_in[:, V_OFF + h * 256:V_OFF + (h + 1) * 256].rearrange("(p j) f -> p j f", j=16), ("ring", slA), slA)
        slB = ring_next()
        wg2 = ringb[slB][:, 0:16 * 512].rearrange("p (a b) -> p a b", a=16)
        wload_into(wg2[:, :, 0:256], w_in[:, GF_OFF + h * 256:GF_OFF + (h + 1) * 256].rearrange("(p j) f -> p j f", j=16), ("ring", slB), slB)
        wload_into(wg2[:, :, 256:512], w_in[:, GB_OFF + h * 256:GB_OFF + (h + 1) * 256].rearrange("(p j) f -> p j f", j=16), ("ring", slB), slB)
        rA = ("ring", slA); rB = ("ring", slB)
        if h == NH - 1:
            pre_m4_sl = ring_next()
            pre_m4_w = wload(pre_m4_sl, 0, wro[:, 0:512].rearrange("(c p) f -> p c f", p=128), (16, 512), ("ring", pre_m4_sl))

        kv_i = [0]

        def tm_kv(srcT, t, rope_tile, wtab, wcols, dst_f, dst_b, dst_v):
            kb_ = kv_i[0] % 2
            kv_i[0] += 1
            for j in range(16):
                mm(PS[kb_][:, 0:384], srcT[:, j, t * 128:(t + 1) * 128], wkv[:, j, :], j == 0, j == 15, ["hT", rA], [("ps", kb_)])
            if rope_tile is None:
                cp("act", krt, PS[kb_][:, 0:128], [("ps", kb_)], ["krt"])
            else:
                cp("act", kb16, PS[kb_][:, 0:128], [("ps", kb_)], ["kb16"])
                tt("dve", krt[:, 0:64], kb16[:, 0:64], cos_tm[:, rope_tile, :], ALU.mult, ["kb16", "cb"], ["krt"])
                tt("dve", krt[:, 64:128], kb16[:, 64:128], cos_tm[:, rope_tile, :], ALU.mult, ["kb16", "cb"], ["krt"])
                tt("dve", kr2[:, 0:64], kb16[:, 64:128], ss_tm[:, rope_tile, 0:64], ALU.mult, ["kb16", "cb"], ["kr2"])
                tt("dve", kr2[:, 64:128], kb16[:, 0:64], ss_tm[:, rope_tile, 64:128], ALU.mult, ["kb16", "cb"], ["kr2"])
                tt("dve", krt, krt, kr2, ALU.add, ["krt", "kr2"], ["krt"])
            nms = wcols if wcols is not None else (("kw", 0), ("kw", 1), "vb")
            ts("dve", dst_f, krt, wtab[0], None, ALU.mult, None, ["krt", "Wtab"], [nms[0]])
            ts("dve", dst_b, krt, wtab[1], None, ALU.mult, None, ["krt", "Wtab"], [nms[1]])
            cp("act", dst_v, PS[kb_][:, 128:384], [("ps", kb_)], [nms[2]])

        if lim >= 1:
            seq = [("c", t) for t in range(NC)] + [("o", t) for t in range(NT)]
            for si, (kind, t) in enumerate(seq):
                if kind == "c":
                    tm_kv(hcT, t, None, (Wc[:, t, h:h + 1], Wc[:, t, 8 + h:9 + h]), None, kwt[0], kwt[1], vbt)
                else:
                    tm_kv(hTo, t, NT + t, (Wo[:, t, h:h + 1], Wo[:, t, 8 + h:9 + h]), None, kwt[0], kwt[1], vbt)
                for d_ in range(2):
                    if _os.environ.get("SKIP_STATE"):
                        continue
                    mm(PS[2 + d_][:, 0:256], kwt[d_], vbt, True, True, [("kw", d_), "vb"], [("ps", 2 + d_)])
                    if si == 0:
                        cp("dve", R32[d_], PS[2 + d_][:, 0:256], [("ps", 2 + d_)], [("R32", d_)])
                    else:
                        tt("dve", R32[d_], R32[d_], PS[2 + d_][:, 0:256], ALU.add, [("R32", d_), ("ps", 2 + d_)], [("R32", d_)])
            for d_ in range(2):
                cp("act", R16[d_], R32[d_], [("R32", d_)], [("R16", d_)])

        if lim >= 2:
            for (wsrc, raw, rot, nm) in ((wq, qraw, qrot, "q"), (wk, kraw, krot, "k")):
                for bi, (t0, tsz) in enumerate(cfg.TB):
                    for j in range(16):
                        mm(PS[4][:, 0:tsz], wsrc[:, j, :], hTw[:, j, t0:t0 + tsz], j == 0, j == 15, [rA, "hT"], [("ps", 4)])
                    cp("act", raw[:, 0:tsz], PS[4][:, 0:tsz], [("ps", 4)], ["raw" + nm])
                    mm(PS[5][:, 0:tsz], pswap, raw[:, 0:tsz], True, True, ["cb", "raw" + nm], [("ps", 5)])
                    tt("dve", rt1[:, 0:tsz], raw[:, 0:tsz], cosT[:, t0:t0 + tsz], ALU.mult, ["raw" + nm, "cb"], ["rt1"])
                    cp("act", sw16[:, 0:tsz], PS[5][:, 0:tsz], [("ps", 5)], ["sw16"])
                    tt("dve", rt2[:, 0:tsz], sw16[:, 0:tsz], sinS[:, t0:t0 + tsz], ALU.mult, ["sw16", "cb"], ["rt2"])
                    tt("dve", rot[:, t0:t0 + tsz], rt1[:, 0:tsz], rt2[:, 0:tsz], ALU.add, ["rt1", "rt2"], ["rot" + nm])
            nch = TOK // 128
            for (dst, q_) in ((qxf, h), (qxb, 8 + h)):
                for c_ in range(nch):
                    tt("dve", dst[:, c_ * 128:(c_ + 1) * 128], qrot[:, c_ * 128:(c_ + 1) * 128], Xi[:, q_, :], ALU.mult,
                       ["rotq", "Xi"], ["qx%d" % (q_ // 8)])

        if lim >= 3:
            for t in range(NT):
                tm_kv(hTw, t, t, (Zt[:, h:h + 1], Zt[:, 8 + h:9 + h]), (("kzf", t), ("kzb", t), ("vown", t)),
                      kzf[:, t, :], kzb[:, t, :], vown[:, t, :])

        if lim >= 4:
            for t in range(NT):
                pb = 4 + (t % 2)
                for j in range(16):
                    mm(PS[pb][:, 0:512], hTw[:, j, t * 128:(t + 1) * 128], wg2[:, j, :], j == 0, j == 15, ["hT", rB], [("ps", pb)])
                act(sgf[:, t, :], PS[pb][:, 0:256], AF.Silu, [("ps", pb)], [("sgf", t)])
                act(sgb[:, t, :], PS[pb][:, 256:512], AF.Silu, [("ps", pb)], [("sgb", t)])

        if lim >= 5:
            for c in range(NT):
                pb = 4 + (c % 2)
                mm(PS[pb][:, 0:128], krot[:, c * 128:(c + 1) * 128], qrot[:, c * 128:(c + 1) * 128], True, True, ["rotk", "rotq"], [("ps", pb)])
                cp("act", st16, PS[pb][:, 0:128], [("ps", pb)], ["st16"])
                tt("dve", SfT[:, c, :], st16, Dm[:, h, :], ALU.mult, ["st16", "Dm"], [("SfT", c)])
                tt("dve", SbT[:, c, :], st16, Dm[:, 8 + h, :], ALU.mult, ["st16", "Dm"], [("SbT", c)])

        if lim >= 6:
            for step in range(NT):
                for d_ in range(2):
                    c = step if d_ == 0 else NT - 1 - step
                    ST = SfT if d_ == 0 else SbT
                    qx = qxf if d_ == 0 else qxb
                    kz = kzf if d_ == 0 else kzb
                    yb = ybf if d_ == 0 else ybb
                    po = d_
                    mm(PS[po][:, 0:256], ST[:, c, :], vown[:, c, :], True, True, [("SfT" if d_ == 0 else "SbT", c), ("vown", c)], [("ps", po)])
                    mm(PS[4 + d_][:, 0:256], qx[:, c * 128:(c + 1) * 128], R16[d_], True, True, ["qx%d" % d_, ("R16", d_)], [("ps", 4 + d_)])
                    ytmp = rt2[:, d_ * 256:(d_ + 1) * 256]
                    cp("dve", ytmp, PS[po][:, 0:256], [("ps", po), "rt2"], ["rt2", ("yt", d_)])
                    tt("dve", ytmp, ytmp, PS[4 + d_][:, 0:256], ALU.add, [("yt", d_), ("ps", 4 + d_)], ["rt2", ("yt", d_)])
                    S.op("dve", lambda e, d_=d_, c=c, ytmp=ytmp: e.bn_stats(out=st6[:, d_ * NT + c, :], in_=ytmp),
                         reads=[("yt", d_)], writes=[("st6", d_, c)])
                    S.op("dve", lambda e, d_=d_, c=c: e.bn_aggr(out=mv[:, d_ * NT + c, :], in_=st6[:, d_ * NT + c, :]),
                         reads=[("st6", d_, c)], writes=[("mv", d_, c)])
                    cp("act", yb[:, c, :], ytmp, [("yt", d_), "rt2"], [("yb", d_, c)])
                    if step < NT - 1:
                        mm(PS[2 + d_][:, 0:256], kz[:, c, :], vown[:, c, :], True, True, [("kzf" if d_ == 0 else "kzb", c), ("vown", c)], [("ps", 2 + d_)])
                        S.op("dve", lambda e, d_=d_, h=h: e.scalar_tensor_tensor(out=R32[d_], in0=R32[d_], scalar=G128[:, d_ * 8 + h:d_ * 8 + h + 1],
                                                                            in1=PS[2 + d_][:, 0:256], op0=ALU.mult, op1=ALU.add),
                             reads=[("R32", d_), ("ps", 2 + d_), "G128"], writes=[("R32", d_)])
                        cp("act", R16[d_], R32[d_], [("R32", d_)], [("R16", d_)])
        if lim >= 7:
            allmv = [("mv", d_, c) for d_ in range(2) for c in range(NT)]
            act(rsd, mv[:, :, 1], AF.Ln, allmv, ["rsd"], bias=EPS)
            act(rsd, rsd, AF.Exp, ["rsd"], ["rsd"], scale=-0.5)
            S.op("dve", lambda e: e.scalar_tensor_tensor(out=nmr, in0=mv[:, :, 0], scalar=-1.0, in1=rsd, op0=ALU.mult, op1=ALU.mult),
                 reads=allmv + ["rsd"], writes=["nmr"])
            for c in range(NT):
                for d_ in range(2):
                    yb = ybf if d_ == 0 else ybb
                    sg = sgf if d_ == 0 else sgb
                    i_ = d_ * NT + c
                    ts("dve", zt[d_], yb[:, c, :], rsd[:, i_:i_ + 1], nmr[:, i_:i_ + 1], ALU.mult, ALU.add,
                       [("yb", d_, c), "rsd", "nmr"], [("zt", d_)])
                    tt("dve", zt[d_], zt[d_], sg[:, c, :], ALU.mult, [("zt", d_), ("sgf" if d_ == 0 else "sgb", c)], [("zt", d_)])
                tt("dve", ybf[:, c, :], zt[0], zt[1], ALU.add, [("zt", 0), ("zt", 1)], [("yb", 0, c)])
                for k2 in range(2):
                    S.op("pe", lambda e, c=c, k2=k2: e.transpose(out=PSB[:, (c % 2) * 1024 + k2 * 128:(c % 2) * 1024 + (k2 + 1) * 128],
                                                                 in_=ybf[:, c, k2 * 128:(k2 + 1) * 128], identity=ident),
                         reads=[("yb", 0, c), "cb"], writes=[("psb", c % 2)])
                for k2 in range(2):
                    cp("act", retT[:, k2, c * 128:(c + 1) * 128], PSB[:, (c % 2) * 1024 + k2 * 128:(c % 2) * 1024 + (k2 + 1) * 128],
                       [("psb", c % 2)], RT_AL)
            for k2 in range(2):
                dma("sp", rscr[2 * h + k2, :, :], retT[:, k2, :], RT_AL, ["rscr"], "retT")
        if lim < 99:
            break
    S.barrier()

    if debug and debug[0] == "stop_m3":
        tmpd = Reg(R_C.base, R_C.size).get(TOK, F32)
        cp("dve", tmpd, retT[:, 0, :], [], ["tmpd"])
        S.barrier()
        dma("sp", dbg_d, tmpd, [], ["dbg"], "dbg")
        S.barrier(); S.emit(); es.close()
        return nc

    acc = arena[:, R_H.base:R_H.base + 2 * NT * D].bitcast(F32).rearrange("p (t f) -> p t f", t=NT)
    R_S.reset()
    rTb = arena[:, R_H.base:R_H.base + 16 * TOK].rearrange("p (j t) -> p j t", j=16)
    mTb = R_S.get(16 * TOK, BF16, (16, TOK))
    mpart = [R_S.get(512, BF16) for _ in range(2)]
    sgt = [R_S.get(512, F32) for _ in range(2)]
    s16 = [R_S.get(512, BF16) for _ in range(2)]
    xres = [R_S.get(512, F32) for _ in range(2)]
    for j in range(16):
        dma("sp", rTb[:, j, :], rscr[j, :, :], [], ["rTb"], "rTb")
    mi = 0
    for ob in range(4):
        if ob == 0:
            slr, wrb = pre_m4_sl, pre_m4_w
        else:
            slr = ring_next()
            wrb = wload(slr, 0, wro[:, ob * 512:(ob + 1) * 512].rearrange("(c p) f -> p c f", p=128), (16, 512), ("ring", slr))
        slm = ring_next()
        wmr = wload(slm, 0, w_in[:, MR_OFF + ob * 512:MR_OFF + (ob + 1) * 512].rearrange("(p j) f -> p j f", j=16), (16, 512), ("ring", slm))
        if ob == 3:
            pre_m5_sl = ring_next()
            pre_m5_w = wload(pre_m5_sl, 0, wout[:, 0:512].rearrange("(c p) f -> p c f", p=128), (16, 512), ("ring", pre_m5_sl))
        for o4 in range(4):
            oc = ob * 4 + o4
            for bi, (t0, tsz) in enumerate(cfg.TB):
                k2 = mi % 2
                pa, pb = k2 * 2, k2 * 2 + 1
                dma("sp", mpart[k2][:, 0:tsz], mscr[oc, :, t0:t0 + tsz], [], [("mpart", k2)], "mp%d" % k2)
                for c in range(16):
                    mm(PS[pa][:, 0:tsz], wrb[:, c, o4 * 128:(o4 + 1) * 128], rTb[:, c, t0:t0 + tsz], c == 0, c == 15, [("ring", slr), "rTb"], [("ps", pa)])
                for j in range(16):
                    mm(PS[pb][:, 0:tsz], wmr[:, j, o4 * 128:(o4 + 1) * 128], hTw[:, j, t0:t0 + tsz], j == 0, j == 15, [("ring", slm), "hT"], [("ps", pb)])
                act(sgt[k2][:, 0:tsz], PS[pb][:, 0:tsz], AF.Sigmoid, [("ps", pb)], [("sgt", k2)])
                tt("dve", s16[k2][:, 0:tsz], PS[pa][:, 0:tsz], sgt[k2][:, 0:tsz], ALU.mult, [("ps", pa), ("sgt", k2)], [("s16", k2)])
                tt("dve", mTb[:, oc, t0:t0 + tsz], s16[k2][:, 0:tsz], mpart[k2][:, 0:tsz], ALU.add, [("s16", k2), ("mpart", k2)], [("mTb", oc)])
                mi += 1
    S.barrier()
    for ob in range(4):
        if ob == 0:
            slo, wob = pre_m5_sl, pre_m5_w
        else:
            slo = ring_next()
            wob = wload(slo, 0, wout[:, ob * 512:(ob + 1) * 512].rearrange("(c p) f -> p c f", p=128), (16, 512), ("ring", slo))
        for t in range(NT):
            k2 = (ob * NT + t) % 2
            pa = 4 + k2
            dma("sp", xres[k2], x_own[t * 128:(t + 1) * 128, ob * 512:(ob + 1) * 512], [], [("xres", k2)], "xr%d" % k2)
            for c in range(16):
                mm(PS[pa][:, :], mTb[:, c, t * 128:(t + 1) * 128], wob[:, c, :], c == 0, c == 15, [("mTb", c), ("ring", slo)], [("ps", pa)])
            tt("dve", acc[:, t, ob * 512:(ob + 1) * 512], PS[pa][:, :], g1b[:, ob * 512:(ob + 1) * 512], ALU.mult,
               [("ps", pa), "g1b"], [("acc", t, ob)])
            tt("dve", acc[:, t, ob * 512:(ob + 1) * 512], acc[:, t, ob * 512:(ob + 1) * 512], xres[k2], ALU.add,
               [("acc", t, ob), ("xres", k2)], [("acc", t, ob)])
    S.barrier()

    if debug and debug[0] == "x1":
        dma("sp", dbg_d.rearrange("(t p) f -> p t f", p=128), acc, [], ["dbg"], "dbg")
        S.barrier(); S.emit(); es.close()
        return nc

    R_S.reset()
    hx2T = R_S.get(16 * TOK, BF16, (16, TOK))
    hidT = [R_S.get(4 * TOK, BF16, (4, TOK)) for _ in range(2)]
    RC = Reg(R_C.base, R_C.size)
    sgm = [RC.get(512, F32) for _ in range(2)]
    cw = RC.get(NT * NE, F32, (NT, NE))
    rw = RC.get(16 * NR, BF16, (16, NR))
    brt = RC.get(NR, F32)
    lgt_all = RC.get(NT * NR, F32, (NT, NR))
    r8 = RC.get(8, F32); m8 = RC.get(8, F32); rt_a = RC.get(8, F32); rt_b = RC.get(8, F32)
    oh = RC.get(NG, F32); gs = RC.get(4, F32)
    dma("pool", rw, w_rt.rearrange("(p j) f -> p j f", j=16), [], ["rw"], "rw")
    dma("sp", brt, b_rt.partition_broadcast(128), [], ["brt"], "c3")
    dma("sp", g1b, fng.partition_broadcast(128), ["g1b"], ["fngb"], "c3")
    xsb2 = [arena[:, R_W.base + i * D:R_W.base + (i + 1) * D] for i in range(2)]
    junk2 = arena[:, R_W.base + 2 * D:R_W.base + 3 * D]
    ssq2 = RC.get(NT, F32)
    pre_wg = wload(1, 0, wg_d[0].rearrange("(p j) f -> p j f", j=16), (16, FF), ("ring", 1))
    pre_wu = wload(2, 0, wu_d[0].rearrange("(p j) f -> p j f", j=16), (16, FF), ("ring", 2))
    def norm_part(t):
        b = t % 2
        accT = acc[:, t, :]
        racc = [("acc", t, ob) for ob in range(4)]
        sq = ssq2[:, t:t + 1]
        act(junk2, accT, AF.Square, racc, ["junk2", ("ssq2", t)], accum_out=sq)
        act(sq, sq, AF.Ln, [("ssq2", t)], [("ssq2", t)], scale=1.0 / D, bias=EPS)
        act(sq, sq, AF.Exp, [("ssq2", t)], [("ssq2", t)], scale=-0.5)
        act(xsb2[b], accT, AF.Copy, racc + [("ssq2", t)], [("xsb2", b)], scale=sq)
        for j in range(16):
            S.op("pe", lambda e, j=j, b=b: e.transpose(out=PSB[:, j * 128:(j + 1) * 128], in_=xsb2[b][:, j::16], identity=ident),
                 reads=[("xsb2", b), "cb"], writes=[("psb", j // 8)])
        for j in range(16):
            ts("dve", hx2T[:, j, t * 128:(t + 1) * 128], PSB[:, j * 128:(j + 1) * 128], A2[:, j:j + 1], B2[:, j:j + 1],
               ALU.mult, ALU.add, [("psb", j // 8), "AB"], [("hx2T", t)])
        for j in range(16):
            mm(PS[4][:, 0:NR], hx2T[:, j, t * 128:(t + 1) * 128], rw[:, j, :], j == 0, j == 15, [("hx2T", t), "rw"], [("ps", 4)])
        tt("dve", lgt_all[:, t, :], PS[4][:, 0:NR], brt, ALU.add, [("ps", 4), "brt"], [("lgt", t)])

    def route_part(t):
        lgt = lgt_all[:, t, :]
        S.op("dve", lambda e: e.memset(r8, -1e30), writes=["r8"])
        cp("dve", r8[:, 0:NG], lgt[:, 0:NG], [("lgt", t), "r8"], ["r8"])
        S.op("dve", lambda e: e.max(out=m8, in_=r8), reads=["r8"], writes=["m8"])
        ts("dve", oh, lgt[:, 0:NG], m8[:, 0:1], None, ALU.is_ge, None, [("lgt", t), "m8"], ["oh"])
        ts("dve", rt_a[:, 0:NG], lgt[:, 0:NG], m8[:, 0:1], None, ALU.subtract, None, [("lgt", t), "m8"], ["rt_a"])
        act(rt_a[:, 0:NG], rt_a[:, 0:NG], AF.Exp, ["rt_a"], ["rt_a"])
        S.op("dve", lambda e: e.reduce_sum(out=gs[:, 0:1], in_=rt_a[:, 0:NG], axis=AX.X), reads=["rt_a"], writes=["gs"])
        S.op("dve", lambda e: e.reciprocal(out=gs[:, 1:2], in_=gs[:, 0:1]), reads=["gs"], writes=["gs"])
        ts("dve", rt_b, lgt[:, NG:NG + 8], oh[:, 0:1], None, ALU.mult, None, [("lgt", t), "oh"], ["rt_b"])
        for g in range(1, NG):
            S.op("dve", lambda e, g=g: e.scalar_tensor_tensor(out=rt_b, in0=lgt[:, NG + 8 * g:NG + 8 * g + 8], scalar=oh[:, g:g + 1],
                                                              in1=rt_b, op0=ALU.mult, op1=ALU.add), reads=[("lgt", t), "oh", "rt_b"], writes=["rt_b"])
        S.op("dve", lambda e: e.max(out=m8, in_=rt_b), reads=["rt_b", "m8"], writes=["m8"])
        ts("dve", rt_a, rt_b, m8[:, 0:1], None, ALU.subtract, None, ["rt_b", "m8"], ["rt_a"])
        act(rt_a, rt_a, AF.Exp, ["rt_a"], ["rt_a"])
        ts("dve", r8, rt_b, m8[:, 1:2], None, ALU.is_ge, None, ["rt_b", "m8", "r8"], ["r8"])
        tt("dve", rt_a, rt_a, r8, ALU.mult, ["rt_a", "r8"], ["rt_a"])
        ts("dve", gs[:, 2:3], m8[:, 1:2], m8[:, 0:1], None, ALU.subtract, None, ["m8", "gs"], ["gs"])
        act(gs[:, 2:3], gs[:, 2:3], AF.Exp, ["gs"], ["gs"])
        ts("dve", gs[:, 2:3], gs[:, 2:3], 1.0, None, ALU.add, None, ["gs"], ["gs"])
        S.op("dve", lambda e: e.reciprocal(out=gs[:, 2:3], in_=gs[:, 2:3]), reads=["gs"], writes=["gs"])
        tt("dve", gs[:, 2:3], gs[:, 2:3], gs[:, 1:2], ALU.mult, ["gs"], ["gs"])
        ts("dve", rt_a, rt_a, gs[:, 2:3], None, ALU.mult, None, ["rt_a", "gs"], ["rt_a"])
        for g in range(NG):
            ts("dve", cw[:, t, 8 * g:8 * g + 8], rt_a, oh[:, g:g + 1], None, ALU.mult, None, ["rt_a", "oh"], [("cw", t)])

    for t in range(NT):
        norm_part(t)
        if t > 0:
            route_part(t - 1)
    route_part(NT - 1)
    S.barrier()
    ringb.append(arena[:, R_K.base:R_K.base + 16 * K])
    NSLOT = 4 if DEAD_END >= 16 * K else 3
    assert NSLOT == 4
    ring_i[0] = 1

    def ring_next4():
        i = ring_i[0] % NSLOT
        ring_i[0] += 1
        return i
    junk3 = hidT[NE % 2][:, 0:2, :].rearrange("p a b -> p (a b)")
    hjunk = ("hid", NE % 2)

    def final_tile(t):
        racc = [("acc", t, ob) for ob in range(4)]
        sq = ssq2[:, t:t + 1]
        act(junk3, acc[:, t, :], AF.Square, racc + [hjunk], [hjunk, ("ssq3", t)], accum_out=sq)
        act(sq, sq, AF.Ln, [("ssq3", t)], [("ssq3", t)], scale=1.0 / D, bias=EPS)
        act(sq, sq, AF.Exp, [("ssq3", t)], [("ssq3", t)], scale=-0.5)
        S.op("dve", lambda e, t=t, sq=sq: e.scalar_tensor_tensor(out=acc[:, t, :], in0=acc[:, t, :], scalar=sq, in1=g1b,
                                                                 op0=ALU.mult, op1=ALU.mult),
             reads=racc + [("ssq3", t), "fngb"], writes=racc)
        dma("sp", out_d[t * 128:(t + 1) * 128, :], acc[:, t, :], racc, ["out"], "outst")

    for ex in range(NE):
        s1, s2, s3 = ring_next4(), ring_next4(), ring_next4()
        if ex == 0:
            assert (s1, s2, s3) == (1, 2, 3)
            wg, wu = pre_wg, pre_wu
        else:
            wg = wload(s1, 0, wg_d[ex].rearrange("(p j) f -> p j f", j=16), (16, FF), ("ring", s1))
            wu = wload(s2, 0, wu_d[ex].rearrange("(p j) f -> p j f", j=16), (16, FF), ("ring", s2))
        wd = wload(s3, 0, wd_d[ex].rearrange("(c p) f -> p c f", p=128), (4, D), ("ring", s3))
        for fc_ in range(4):
            tt("dve", wd[:, fc_, :], wd[:, fc_, :], g2b, ALU.mult, [("ring", s3), "g2b"], [("ring", s3)])
        hb = hidT[ex % 2]
        hn = ("hid", ex % 2)
        mi = 0
        for bi, (t0, tsz) in enumerate(cfg.TB):
            for fc in range(4):
                k2 = mi % 2
                pa, pb = k2 * 2, k2 * 2 + 1
                for j in range(16):
                    mm(PS[pa][:, 0:tsz], wg[:, j, fc * 128:(fc + 1) * 128], hx2T[:, j, t0:t0 + tsz], j == 0, j == 15, [("ring", s1), "hx2"], [("ps", pa)])
                for j in range(16):
                    mm(PS[pb][:, 0:tsz], wu[:, j, fc * 128:(fc + 1) * 128], hx2T[:, j, t0:t0 + tsz], j == 0, j == 15, [("ring", s2), "hx2"], [("ps", pb)])
                act(sgm[k2][:, 0:tsz], PS[pa][:, 0:tsz], AF.Silu, [("ps", pa)], [("sgm", k2)])
                tt("dve", hb[:, fc, t0:t0 + tsz], PS[pb][:, 0:tsz], sgm[k2][:, 0:tsz], ALU.mult, [("ps", pb), ("sgm", k2)], [hn])
                mi += 1
        for t in range(NT):
            for ob in range(4):
                pa = 4 + (t * 4 + ob) % 2
                for fc in range(4):
                    mm(PS[pa][:, :], hb[:, fc, t * 128:(t + 1) * 128], wd[:, fc, ob * 512:(ob + 1) * 512], fc == 0, fc == 3, [hn, ("ring", s3)], [("ps", pa)])
                S.op("dve", lambda e, t=t, ob=ob, pa=pa, ex=ex: e.scalar_tensor_tensor(
                    out=acc[:, t, ob * 512:(ob + 1) * 512], in0=PS[pa][:, :], scalar=cw[:, t, ex:ex + 1],
                    in1=acc[:, t, ob * 512:(ob + 1) * 512], op0=ALU.mult, op1=ALU.add),
                    reads=[("ps", pa), ("cw", t), ("acc", t, ob)], writes=[("acc", t, ob)])
            if ex == NE - 1 and t > 0:
                final_tile(t - 1)
    final_tile(NT - 1)
    S.barrier()
    S.emit()
    es.close()
    return nc


def make_in_maps(cfg, x, c, ctx, c_ctx, w_mod, b_mod, norm1_g, norm2_g, w_in, w_four_out, w_ret_out, w_out,
                 ret_decay_f, ret_decay_b, w_group_router, b_group_router, w_expert_router, b_expert_router,
                 w_gate, w_up, w_down, final_norm_g):
    f32 = lambda a: np.ascontiguousarray(np.asarray(a, dtype=np.float32))
    x = f32(x); ctx = f32(ctx); c = f32(c)
    NG = cfg.NG
    w_rt = np.concatenate([f32(w_group_router[0]), f32(w_expert_router[0]).transpose(1, 0, 2).reshape(D, NG * 8)], axis=1)
    b_rt = np.concatenate([f32(b_group_router[0]), f32(b_expert_router[0]).reshape(-1)])
    decay = np.concatenate([f32(ret_decay_f[0]), f32(ret_decay_b[0])])
    shared = {
        "c_ctx": f32(c_ctx), "w_mod": f32(w_mod[0]), "b_mod": f32(b_mod[0]), "norm1_g": f32(norm1_g[0]), "norm2_g": f32(norm2_g[0]),
        "final_norm_g": f32(final_norm_g), "w_in": f32(w_in[0]), "w_four_out": f32(w_four_out[0]), "w_ret_out": f32(w_ret_out[0]),
        "w_out": f32(w_out[0]), "decay": decay, "w_router": np.ascontiguousarray(w_rt), "b_router": np.ascontiguousarray(b_rt),
        "w_gate": f32(w_gate[0]), "w_up": f32(w_up[0]), "w_down": f32(w_down[0]),
    }
    tabs = [host_tables(cfg, half) for half in range(2)]
    maps = []
    TOK = cfg.TOK
    for b in range(cfg.B):
        for half in range(2):
            dft, cb, cf = tabs[half]
            m = dict(shared)
            m["x_own"] = np.ascontiguousarray(x[b, half * TOK:(half + 1) * TOK])
            m["x_oth"] = np.ascontiguousarray(x[b, (1 - half) * TOK:(2 - half) * TOK])
            m["ctx"] = np.ascontiguousarray(ctx[b])
            m["cvec"] = np.ascontiguousarray(c[b])
            m["dft"] = dft; m["cbf"] = cb; m["cf32"] = cf
            maps.append(m)
    return maps


_CACHE = {}


def kernel(**inputs):
    cfg = Cfg()
    if "nc" not in _CACHE:
        _CACHE["nc"] = build(cfg)
    maps = make_in_maps(cfg, **inputs)
    res = run_bass_kernel_spmd(_CACHE["nc"], maps, core_ids=list(range(8)))
    out = np.empty((cfg.B, cfg.SEQ, D), np.float32)
    for b in range(cfg.B):
        for half in range(2):
            out[b, half * cfg.TOK:(half + 1) * cfg.TOK] = res.results[b * 2 + half]["out"]
    return out
```
